# Optimizing a Trainium2 kernel written in Bass

```python
import math
import jax, jax.numpy as jnp
from jax import lax
import numpy as np

D_MODEL = 2048
BATCH = 4
SEQ = 8192
DEPTH = 1
DEC_BATCH = 32
DEC_SEQ = 64
PAST_LEN = 4096

CHUNK = 64
BLOCK_Q = 128
D_MIX = D_MODEL
SB_HEADS = 8
SB_HD = 128
SB_WIDTH = SB_HEADS * SB_HD
DA_HEADS = 4
DA_HD = 128
DA_VD = 2 * DA_HD
DA_QK_WIDTH = DA_HEADS * 2 * DA_HD
DA_WIDTH = DA_HEADS * DA_VD
D_IN_PROJ = 3 * SB_WIDTH + 2 * DA_QK_WIDTH + DA_WIDTH
SPLIT_POINTS = [SB_WIDTH, 2 * SB_WIDTH, 3 * SB_WIDTH,
                3 * SB_WIDTH + DA_QK_WIDTH, 3 * SB_WIDTH + 2 * DA_QK_WIDTH]
N_GROUPS = 4
EXPERTS_PER_GROUP = 4
N_EXPERTS = N_GROUPS * EXPERTS_PER_GROUP
TOP_K = 2
D_EXPERT = D_MODEL // 4
LN_EPS = 1e-5
RMS_EPS = 1e-5
NEG_INF = -1e30
ALPHA = (2 * DEPTH) ** 0.25
BETA = (8 * DEPTH) ** -0.25

kernel_name = "hybrid_stickbreak_diffattn_hmoe_stream_step"


def _layer_norm(x, g, b):
    xf = x.astype(jnp.float32)
    mu = jnp.mean(xf, axis=-1, keepdims=True)
    var = jnp.mean(jnp.square(xf - mu), axis=-1, keepdims=True)
    y = (xf - mu) * lax.rsqrt(var + LN_EPS) * g.astype(jnp.float32) + b.astype(jnp.float32)
    return y.astype(x.dtype)


def _rms_norm(x, g):
    xf = x.astype(jnp.float32)
    y = xf * lax.rsqrt(jnp.mean(jnp.square(xf), axis=-1, keepdims=True) + RMS_EPS)
    return (y * g.astype(jnp.float32)).astype(x.dtype)


def _alibi_slopes(n):
    return jnp.asarray([2.0 ** (-8.0 * (h + 1) / n) for h in range(n)], dtype=jnp.float32)


def _stick_breaking_block(q, k, v, q_pos, k_pos):
    z = jnp.einsum("bqhd,bshd->bhqs", q, k).astype(jnp.float32) * (SB_HD ** -0.5)
    mask = k_pos[None, :] < q_pos[:, None]
    log_not = jnp.where(mask, jax.nn.log_sigmoid(-z), 0.0)
    between = lax.cumsum(log_not, axis=3, reverse=True) - log_not
    w = jnp.where(mask, jnp.exp(jax.nn.log_sigmoid(z) + between), 0.0)
    return jnp.einsum("bhqs,bshd->bqhd", w.astype(v.dtype), v)


def _diff_attn_block(q, k, v, q_pos, k_pos, lam, slopes):
    s = jnp.einsum("bqhmd,bshmd->bhmqs", q, k).astype(jnp.float32) * (DA_HD ** -0.5)
    dist = jnp.abs(q_pos[:, None] - k_pos[None, :]).astype(jnp.float32)
    visible = (k_pos[None, :] // CHUNK) <= (q_pos[:, None] // CHUNK)
    s = jnp.where(visible, s - slopes[None, :, None, None, None] * dist, NEG_INF)
    p = jax.nn.softmax(s, axis=-1)
    w = p[:, :, 0] - lam * p[:, :, 1]
    return jnp.einsum("bhqs,bshv->bqhv", w.astype(v.dtype), v)


def _query_blocks(fn, q, q_pos):
    b, s = q.shape[0], q.shape[1]
    if s <= BLOCK_Q:
        return fn(q, q_pos)
    nb = s // BLOCK_Q
    qb = jnp.moveaxis(q.reshape((b, nb, BLOCK_Q) + q.shape[2:]), 1, 0)
    pb = q_pos.reshape(nb, BLOCK_Q)
    out = lax.map(lambda a: fn(a[0], a[1]), (qb, pb))
    return jnp.moveaxis(out, 0, 1).reshape((b, s) + out.shape[3:])


def _mixer(x, q_pos, k_pos, past, w_in, w_out, lq1, lk1, lq2, lk2, sub_g, lam_init, slopes):
    b, s, _ = x.shape
    proj = jnp.einsum("bsd,de->bse", x, w_in)
    sq, sk, sv, dq, dk, dv = jnp.split(proj, SPLIT_POINTS, axis=-1)
    sq = sq.reshape(b, s, SB_HEADS, SB_HD)
    sk = sk.reshape(b, s, SB_HEADS, SB_HD)
    sv = sv.reshape(b, s, SB_HEADS, SB_HD)
    dq = dq.reshape(b, s, DA_HEADS, 2, DA_HD)
    dk = dk.reshape(b, s, DA_HEADS, 2, DA_HD)
    dv = dv.reshape(b, s, DA_HEADS, DA_VD)
    new_rows = (sk, sv, dk, dv)
    if past is not None:
        sk = jnp.concatenate([past[0].astype(sk.dtype), sk], axis=1)
        sv = jnp.concatenate([past[1].astype(sv.dtype), sv], axis=1)
        dk = jnp.concatenate([past[2].astype(dk.dtype), dk], axis=1)
        dv = jnp.concatenate([past[3].astype(dv.dtype), dv], axis=1)
    lam = (jnp.exp(jnp.sum(lq1.astype(jnp.float32) * lk1.astype(jnp.float32)))
           - jnp.exp(jnp.sum(lq2.astype(jnp.float32) * lk2.astype(jnp.float32))) + lam_init)
    sb_o = _query_blocks(lambda qb, pb: _stick_breaking_block(qb, sk, sv, pb, k_pos), sq, q_pos)
    da_o = _query_blocks(lambda qb, pb: _diff_attn_block(qb, dk, dv, pb, k_pos, lam, slopes), dq, q_pos)
    da_o = _rms_norm(da_o, sub_g) * (1.0 - lam_init)
    merged = jnp.concatenate([sb_o.reshape(b, s, SB_WIDTH), da_o.reshape(b, s, DA_WIDTH)], axis=-1)
    return jnp.einsum("bse,ed->bsd", merged, w_out), new_rows


def _hmoe(x, w_coarse, b_coarse, w_fine, b_fine, w_gate, w_up, w_down):
    b, s, d = x.shape
    xt = x.reshape(b * s, d)
    coarse = (xt @ w_coarse + b_coarse).astype(jnp.float32)
    p_group = jax.nn.softmax(coarse, axis=-1)
    g_sel = jnp.argmax(coarse, axis=-1)
    g_gate = jnp.max(p_group, axis=-1)
    g_onehot = jax.nn.one_hot(g_sel, N_GROUPS, dtype=jnp.float32)
    fine = (jnp.einsum("td,dge->tge", xt, w_fine) + b_fine).astype(jnp.float32)
    fine_sel = jnp.einsum("tge,tg->te", fine, g_onehot)
    top_v, top_i = lax.top_k(fine_sel, TOP_K)
    top_w = jax.nn.softmax(top_v, axis=-1) * g_gate[:, None]
    within = jnp.sum(jax.nn.one_hot(top_i, EXPERTS_PER_GROUP, dtype=jnp.float32) * top_w[..., None], axis=1)
    comb = (g_onehot[:, :, None] * within[:, None, :]).reshape(b * s, N_EXPERTS).astype(x.dtype)
    y = jnp.zeros_like(xt)
    for e in range(N_EXPERTS):
        h = jax.nn.silu(xt @ w_gate[e]) * (xt @ w_up[e])
        y = y + comb[:, e:e + 1] * (h @ w_down[e])
    return y.reshape(b, s, d)


def setup_inputs(seed: int = 0) -> dict:
    key = jax.random.key(seed)
    ks = jax.random.split(key, 24)
    f32 = jnp.float32
    col_scale = jnp.concatenate([
        jnp.ones((2 * SB_WIDTH,), f32), BETA * jnp.ones((SB_WIDTH,), f32),
        jnp.ones((2 * DA_QK_WIDTH,), f32), BETA * jnp.ones((DA_WIDTH,), f32)])
    return {
        "x_prompt": jax.random.normal(ks[0], (BATCH, SEQ, D_MODEL), f32),
        "x_sample": jax.random.normal(ks[1], (DEC_BATCH, DEC_SEQ, D_MODEL), f32),
        "cache_sb_k": jax.random.normal(ks[2], (DEPTH, DEC_BATCH, PAST_LEN, SB_HEADS, SB_HD), f32),
        "cache_sb_v": BETA * jax.random.normal(ks[3], (DEPTH, DEC_BATCH, PAST_LEN, SB_HEADS, SB_HD), f32),
        "cache_da_k": jax.random.normal(ks[4], (DEPTH, DEC_BATCH, PAST_LEN, DA_HEADS, 2, DA_HD), f32),
        "cache_da_v": BETA * jax.random.normal(ks[5], (DEPTH, DEC_BATCH, PAST_LEN, DA_HEADS, DA_VD), f32),
        "w_in": jax.random.normal(ks[6], (DEPTH, D_MODEL, D_IN_PROJ), f32) * (D_MODEL ** -0.5) * col_scale,
        "w_out": jax.random.normal(ks[7], (DEPTH, D_MIX, D_MODEL), f32) * (D_MIX ** -0.5) * BETA,
        "lambda_q1": 0.1 * jax.random.normal(ks[8], (DEPTH, DA_HD), f32),
        "lambda_k1": 0.1 * jax.random.normal(ks[9], (DEPTH, DA_HD), f32),
        "lambda_q2": 0.1 * jax.random.normal(ks[10], (DEPTH, DA_HD), f32),
        "lambda_k2": 0.1 * jax.random.normal(ks[11], (DEPTH, DA_HD), f32),
        "subln_g": 1.0 + 0.02 * jax.random.normal(ks[12], (DEPTH, DA_VD), f32),
        "ln1_g": 1.0 + 0.02 * jax.random.normal(ks[13], (DEPTH, D_MODEL), f32),
        "ln1_b": 0.02 * jax.random.normal(ks[14], (DEPTH, D_MODEL), f32),
        "w_coarse": jax.random.normal(ks[15], (DEPTH, D_MODEL, N_GROUPS), f32) * (D_MODEL ** -0.5),
        "b_coarse": 0.01 * jax.random.normal(ks[16], (DEPTH, N_GROUPS), f32),
        "w_fine": jax.random.normal(ks[17], (DEPTH, D_MODEL, N_GROUPS, EXPERTS_PER_GROUP), f32) * (D_MODEL ** -0.5),
        "b_fine": 0.01 * jax.random.normal(ks[18], (DEPTH, N_GROUPS, EXPERTS_PER_GROUP), f32),
        "w_gate": jax.random.normal(ks[19], (DEPTH, N_EXPERTS, D_MODEL, D_EXPERT), f32) * (D_MODEL ** -0.5),
        "w_up": jax.random.normal(ks[20], (DEPTH, N_EXPERTS, D_MODEL, D_EXPERT), f32) * (D_MODEL ** -0.5) * BETA,
        "w_down": jax.random.normal(ks[21], (DEPTH, N_EXPERTS, D_EXPERT, D_MODEL), f32) * (D_EXPERT ** -0.5) * BETA,
        "ln2_g": 1.0 + 0.02 * jax.random.normal(ks[22], (DEPTH, D_MODEL), f32),
        "ln2_b": 0.02 * jax.random.normal(ks[23], (DEPTH, D_MODEL), f32),
    }


def reference(x_prompt, x_sample, cache_sb_k, cache_sb_v, cache_da_k, cache_da_v,
              w_in, w_out, lambda_q1, lambda_k1, lambda_q2, lambda_k2, subln_g,
              ln1_g, ln1_b, w_coarse, b_coarse, w_fine, b_fine, w_gate, w_up, w_down,
              ln2_g, ln2_b):
    slopes = _alibi_slopes(DA_HEADS)
    s_p = x_prompt.shape[1]
    s_s = x_sample.shape[1]
    past_len = cache_sb_k.shape[2]
    pos_p = jnp.arange(s_p, dtype=jnp.int32)
    pos_s = past_len + jnp.arange(s_s, dtype=jnp.int32)
    kpos_s = jnp.arange(past_len + s_s, dtype=jnp.int32)
    hp, hs = x_prompt, x_sample
    rows_p, rows_s = [], []
    for l in range(DEPTH):
        lam_init = 0.8 - 0.6 * math.exp(-0.3 * l)
        mix_w = (w_in[l], w_out[l], lambda_q1[l], lambda_k1[l], lambda_q2[l], lambda_k2[l],
                 subln_g[l], lam_init, slopes)
        a_p, r_p = _mixer(hp, pos_p, pos_p, None, *mix_w)
        past = (cache_sb_k[l], cache_sb_v[l], cache_da_k[l], cache_da_v[l])
        a_s, r_s = _mixer(hs, pos_s, kpos_s, past, *mix_w)
        hp = _layer_norm(ALPHA * hp + a_p, ln1_g[l], ln1_b[l])
        hs = _layer_norm(ALPHA * hs + a_s, ln1_g[l], ln1_b[l])
        moe_w = (w_coarse[l], b_coarse[l], w_fine[l], b_fine[l], w_gate[l], w_up[l], w_down[l])
        hp = _layer_norm(ALPHA * hp + _hmoe(hp, *moe_w), ln2_g[l], ln2_b[l])
        hs = _layer_norm(ALPHA * hs + _hmoe(hs, *moe_w), ln2_g[l], ln2_b[l])
        rows_p.append(r_p)
        rows_s.append(r_s)
    new_sb_k_prompt = jnp.stack([r[0] for r in rows_p], axis=0)
    new_sb_v_prompt = jnp.stack([r[1] for r in rows_p], axis=0)
    new_da_k_prompt = jnp.stack([r[2] for r in rows_p], axis=0)
    new_da_v_prompt = jnp.stack([r[3] for r in rows_p], axis=0)
    new_sb_k_sample = jnp.stack([r[0] for r in rows_s], axis=0)
    new_sb_v_sample = jnp.stack([r[1] for r in rows_s], axis=0)
    new_da_k_sample = jnp.stack([r[2] for r in rows_s], axis=0)
    new_da_v_sample = jnp.stack([r[3] for r in rows_s], axis=0)
    return (hp, hs, new_sb_k_prompt, new_sb_v_prompt, new_da_k_prompt, new_da_v_prompt,
            new_sb_k_sample, new_sb_v_sample, new_da_k_sample, new_da_v_sample)
```

```python
from contextlib import ExitStack
import numpy as np
import ml_dtypes
import concourse.bass as bass
import concourse.mybir as mybir
from concourse.bass_utils import run_bass_kernel_spmd

F32 = mybir.dt.float32
F32R = mybir.dt.float32r
BF16 = mybir.dt.bfloat16
AF = mybir.ActivationFunctionType
ALU = mybir.AluOpType
AX = mybir.AxisListType

D = 2048
DIN = 6144
NEGM = -30000.0
ALPHA = 2.0 ** 0.25
LN_EPS = 1e-5
RMS_EPS = 1e-5
LAM_INIT = 0.8 - 0.6
SLOPES = [2.0 ** (-8.0 * (h + 1) / 4) for h in range(4)]
QSCALE = 128.0 ** -0.5


class Cfg:
    def __init__(self, S=8192, PAST=4096, NSB=4, stages=("proj", "sb", "da", "ffn"), debug=False):
        self.S = S
        self.PAST = PAST
        self.NSB = NSB
        self.DS = 64
        self.NL = S // 1024
        self.NOWN = S // 2
        self.NTOK = self.NOWN + NSB * 64
        self.stages = stages
        self.debug = debug


class Buf:
    __slots__ = ("name", "w", "r")

    def __init__(self, name=""):
        self.name = name
        self.w = {}
        self.r = {}


class Eng:
    def __init__(self, kb, eng, name, skip_self=False):
        self.kb = kb
        self.eng = eng
        self.name = name
        self.sem = kb.es.enter_context(kb.nc.semaphore("sem_" + name))
        self.cnt = 0
        self.seen = {}
        self.skip_self = skip_self

    def wait(self, evs):
        for sem, val in evs.items():
            if sem is self.sem and self.skip_self:
                continue
            if self.seen.get(sem, 0) < val:
                self.eng.wait_ge(sem, val)
                self.seen[sem] = val

    def op(self, fn, reads=(), writes=()):
        deps = {}
        for b in reads:
            for s, v in b.w.items():
                if deps.get(s, 0) < v:
                    deps[s] = v
        for b in writes:
            for dd in (b.w, b.r):
                for s, v in dd.items():
                    if deps.get(s, 0) < v:
                        deps[s] = v
        self.wait(deps)
        ins = fn(self.eng)
        self.cnt += 1
        ins.then_inc(self.sem, 1)
        for b in reads:
            b.r[self.sem] = self.cnt
        for b in writes:
            b.w = {self.sem: self.cnt}
            b.r = {}
        return ins


class KB:
    NDS = 40

    def __init__(self):
        self.nc = bass.Bass("TRN2", target_bir_lowering=False)
        self.es = ExitStack()
        nc = self.nc
        self.pe = Eng(self, nc.tensor, "pe", skip_self=True)
        self.act = Eng(self, nc.scalar, "act")
        self.dve = Eng(self, nc.vector, "dve")
        self.pool = Eng(self, nc.gpsimd, "pool")
        self.sp = Eng(self, nc.sync, "sp")
        self.engs = [self.pe, self.act, self.dve, self.pool, self.sp]
        self.dsems = [self.es.enter_context(nc.semaphore("dsem%d" % i)) for i in range(self.NDS)]
        self.duse = [0] * self.NDS
        self.di = 0

    def dma(self, q, out, in_, reads=(), writes=()):
        k = self.di % self.NDS
        self.di += 1
        sem = self.dsems[k]
        deps = {}
        if self.duse[k] > 0:
            deps[sem] = 16 * self.duse[k]
        for b in reads:
            for s, v in b.w.items():
                if deps.get(s, 0) < v:
                    deps[s] = v
        for b in writes:
            for dd in (b.w, b.r):
                for s, v in dd.items():
                    if deps.get(s, 0) < v:
                        deps[s] = v
        q.wait(deps)
        q.eng.dma_start(out=out, in_=in_).then_inc(sem, 16)
        self.duse[k] += 1
        val = 16 * self.duse[k]
        for b in reads:
            b.r[sem] = val
        for b in writes:
            b.w = {sem: val}
            b.r = {}

    def barrier(self):
        evs = {}
        for e in self.engs:
            if e.cnt > 0:
                evs[e.sem] = e.cnt
        for k in range(self.NDS):
            if self.duse[k] > 0:
                evs[self.dsems[k]] = 16 * self.duse[k]
        for e in self.engs:
            sk = e.skip_self
            e.skip_self = False
            e.wait(evs)
            e.skip_self = sk


class T:
    def __init__(self, t, name):
        self.t = t
        self.b = Buf(name)

    def __getitem__(self, idx):
        return self.t[idx]


def sbt(kb, stack, name, shape, dt):
    return T(stack.enter_context(kb.nc.sbuf_tensor(name, list(shape), dt)), name)


def pst(kb, stack, name, shape, dt):
    return T(stack.enter_context(kb.nc.psum_tensor(name, list(shape), dt)), name)


def build(cfg):
    kb = KB()
    nc = kb.nc
    pe, act, dve, pool, sp = kb.pe, kb.act, kb.dve, kb.pool, kb.sp
    S, PAST, NSB, NL, NOWN, NTOK = cfg.S, cfg.PAST, cfg.NSB, cfg.NL, cfg.NOWN, cfg.NTOK
    NS = NSB * 64
    NKBP = S // 128

    def din(name, shape, dt=F32):
        return nc.dram_tensor(name, list(shape), dt, kind="ExternalInput").ap()

    def dout(name, shape, dt=F32):
        return nc.dram_tensor(name, list(shape), dt, kind="ExternalOutput").ap()

    def dscr(name, shape, dt=BF16):
        return nc.dram_tensor(name, list(shape), dt, kind="Internal").ap()

    xf = din("xf", [S, D])
    xo = din("xo", [NOWN, D])
    xs = din("xs", [NS, D])
    csk = din("csk", [NSB, PAST, 1024])
    csv = din("csv", [NSB, PAST, 1024])
    cdk = din("cdk", [NSB, PAST, 1024])
    cdv = din("cdv", [NSB, PAST, 1024])
    w_in = din("w_in", [D, DIN])
    if "ffn" in cfg.stages:
        w_out = din("w_out", [D, D])
        w_gate = din("w_gate", [16, D, 512])
        w_up = din("w_up", [16, D, 512])
        w_down = din("w_down", [16, 512, D])
        w_r = din("w_r", [D, 20])
        sel = din("sel", [16, 16, 128])
    NV = 20 + 4 * 128 + 2
    vecs = din("vecs", [128, NV])
    if "ffn" in cfg.stages:
        lnv = din("lnv", [128, 4 * D])
    tabs = din("tabs", [128, 128 * 4], F32)
    sbmask = din("sbmask", [128, 8, 512], BF16)
    damask = din("damask", [128, 4, 8, 512], BF16)
    sbmask_s = din("sbmask_s", [64, 512], BF16)
    damask_s = din("damask_s", [64, 512], BF16)
    atab = din("atab", [4, 72, 128], BF16)
    NKS = PAST // 128 + 1
    atab_s = din("atab_s", [4, NKS, 128], BF16)
    btab = din("btab", [4, 4, 512], BF16)
    btab_s = din("btab_s", [4, 512], BF16)

    y_out = dout("y_out", [NTOK, D])
    kv_p = dout("kv_p", [S, 4096])
    kv_s = dout("kv_s", [NS, 4096])

    kt_scr = dscr("kt_scr", [16, 128, S])
    v_scr = dscr("v_scr", [S, 2048])
    qt_scr = dscr("qt_scr", [16, 128, NOWN])
    kts_scr = dscr("kts_scr", [16, 128, NS])
    vs_scr = dscr("vs_scr", [NS, 2048])
    qts_scr = dscr("qts_scr", [16, 128, NS])
    if cfg.debug:
        mt_scr = dout("mt_scr", [16, 128, NTOK], BF16)
    else:
        mt_scr = dscr("mt_scr", [16, 128, NTOK])
    scrbuf = Buf("scratch")

    es = kb.es
    ident_f = sbt(kb, es, "ident_f", [128, 128], F32)
    tri_f = sbt(kb, es, "tri_f", [128, 128], F32)
    ones_f = sbt(kb, es, "ones_f", [128, 128], F32)
    ident_b = sbt(kb, es, "ident_b", [128, 128], BF16)
    ones_b = sbt(kb, es, "ones_b", [128, 128], BF16)
    vec_t = sbt(kb, es, "vec_t", [128, NV], F32)
    kb.dma(sp, ident_f[:], tabs[:, 0:128], writes=[ident_f.b])
    kb.dma(sp, tri_f[:], tabs[:, 128:256], writes=[tri_f.b])
    kb.dma(sp, ones_f[:], tabs[:, 256:384], writes=[ones_f.b])
    kb.dma(sp, vec_t[:], vecs[:, :], writes=[vec_t.b])
    dve.op(lambda e: e.tensor_copy(out=ident_b[:], in_=ident_f[:]), reads=[ident_f.b], writes=[ident_b.b])
    dve.op(lambda e: e.tensor_copy(out=ones_b[:], in_=ones_f[:]), reads=[ones_f.b], writes=[ones_b.b])
    tri_r = sbt(kb, es, "tri_r", [128, 128], F32)
    ones_r = sbt(kb, es, "ones_r", [128, 128], F32)
    dve.op(lambda e: e.tensor_copy(out=tri_r[:].bitcast(F32R), in_=tri_f[:]), reads=[tri_f.b], writes=[tri_r.b])
    dve.op(lambda e: e.tensor_copy(out=ones_r[:].bitcast(F32R), in_=ones_f[:]), reads=[ones_f.b], writes=[ones_r.b])

    def phase_proj():
        with ExitStack() as st:
            TG = 1024
            xb = [sbt(kb, st, "xb%d" % i, [128, D], BF16) for i in range(2)]
            xTs = [sbt(kb, st, "xT%d" % i, [128, 16, TG], BF16) for i in range(2)]
            wb = [sbt(kb, st, "wb%d" % i, [128, 16, 512], BF16) for i in range(2)]
            stf = [sbt(kb, st, "stf%d" % i, [128, 512], F32) for i in range(3)]
            stb = [sbt(kb, st, "stb%d" % i, [128, 512], BF16) for i in range(3)]
            tst = [sbt(kb, st, "tst%d" % i, [128, 4, TG], BF16) for i in range(2)]
            pacc = [pst(kb, st, "pacc%d" % i, [128, 512], F32) for i in range(3)]
            ptr = [pst(kb, st, "ptr%d" % i, [128, 1024], BF16) for i in range(2)]
            cnt = {"x": 0, "w": 0, "t": 0, "tr": 0, "ts": 0}

            def load_xT(src, ntok, xT):
                for tt in range(ntok // 128):
                    xbi = xb[cnt["x"] % 2]
                    cnt["x"] += 1
                    kb.dma(pool, xbi[:], src[tt * 128:(tt + 1) * 128, :], writes=[xbi.b])
                    for half in range(2):
                        p = ptr[cnt["tr"] % 2]
                        cnt["tr"] += 1
                        for j in range(8):
                            c = half * 8 + j
                            pe.op(lambda e, p=p, j=j, c=c: e.transpose(p[:, j * 128:(j + 1) * 128],
                                                                       xbi[:, c * 128:(c + 1) * 128], ident_b[:]),
                                  reads=[xbi.b, ident_b.b], writes=[p.b])
                        ev = act if (cnt["tr"] % 2) else dve
                        if ev is act:
                            ev.op(lambda e, p=p, half=half, tt=tt: e.copy(
                                out=xT[:, half * 8:(half + 1) * 8, tt * 128:(tt + 1) * 128],
                                in_=p[:].rearrange("p (c t) -> p c t", c=8)), reads=[p.b], writes=[xT.b])
                        else:
                            ev.op(lambda e, p=p, half=half, tt=tt: e.tensor_copy(
                                out=xT[:, half * 8:(half + 1) * 8, tt * 128:(tt + 1) * 128],
                                in_=p[:].rearrange("p (c t) -> p c t", c=8)), reads=[p.b], writes=[xT.b])

            def proj_cols(ntok, cts, sink_tok, sink_T, xT, hook=None):
                pend = []

                def wload(ct):
                    wbi = wb[cnt["w"] % 2]
                    cnt["w"] += 1
                    kb.dma(pool, wbi[:], w_in.rearrange("(c p) n -> p c n", p=128)[:, :, ct * 512:(ct + 1) * 512],
                           writes=[wbi.b])
                    return wbi
                wq = [wload(cts[0])]
                for ici, ct in enumerate(cts):
                    if ici == 1 and hook is not None:
                        hook()
                    wbi = wq.pop(0)
                    if ici + 1 < len(cts):
                        wq.append(wload(cts[ici + 1]))
                    kind = sink_tok(ct)
                    tsi = None
                    if kind["T"]:
                        tsi = tst[cnt["ts"] % 2]
                        cnt["ts"] += 1
                    for tt in range(ntok // 128):
                        i = cnt["t"]
                        cnt["t"] += 1
                        pa = pacc[i % 3]
                        for c in range(16):
                            pe.op(lambda e, pa=pa, c=c, tt=tt: e.matmul(pa[:], lhsT=xT[:, c, tt * 128:(tt + 1) * 128],
                                                                         rhs=wbi[:, c, :], start=(c == 0), stop=(c == 15)),
                                  reads=[xT.b, wbi.b], writes=[pa.b])
                        sf, sb_ = stf[i % 3], stb[i % 3]
                        if kind["f32"] is not None:
                            act.op(lambda e, sf=sf, pa=pa: e.copy(out=sf[:], in_=pa[:]), reads=[pa.b], writes=[sf.b])
                            kb.dma(sp, kind["f32"](tt), sf[:], reads=[sf.b], writes=[scrbuf])
                            pool.op(lambda e, sf=sf, sb_=sb_: e.tensor_copy(out=sb_[:], in_=sf[:]),
                                    reads=[sf.b], writes=[sb_.b])
                        else:
                            act.op(lambda e, sb_=sb_, pa=pa: e.activation(out=sb_[:], in_=pa[:], func=AF.Copy,
                                                                          scale=kind["scale"]),
                                   reads=[pa.b], writes=[sb_.b])
                        if kind["b16"] is not None:
                            kb.dma(sp, kind["b16"](tt), sb_[:], reads=[sb_.b], writes=[scrbuf])
                        for f in pend:
                            f()
                        pend = []
                        if kind["T"]:
                            def do_T(sb_=sb_, tsi=tsi, tt=tt):
                                p = ptr[cnt["tr"] % 2]
                                cnt["tr"] += 1
                                for j in range(4):
                                    pe.op(lambda e, j=j: e.transpose(p[:, j * 128:(j + 1) * 128],
                                                                     sb_[:, j * 128:(j + 1) * 128], ident_b[:]),
                                          reads=[sb_.b, ident_b.b], writes=[p.b])
                                dve.op(lambda e: e.tensor_copy(out=tsi[:, :, tt * 128:(tt + 1) * 128],
                                                               in_=p[:, 0:512].rearrange("p (h t) -> p h t", h=4)),
                                       reads=[p.b], writes=[tsi.b])
                            pend.append(do_T)
                    for f in pend:
                        f()
                    pend = []
                    if kind["T"]:
                        sink_T(ct, tsi)

            OUTC = {2: 0, 3: 512, 4: 1024, 5: 1536, 8: 2048, 9: 2560, 10: 3072, 11: 3584}
            KTH = {2: 0, 3: 4, 8: 8, 9: 12}
            QTH = {0: 0, 1: 4, 6: 8, 7: 12}
            VC = {4: 0, 5: 512, 10: 1024, 11: 1536}

            def mk_sinks(tok0, ntok, out_ap, kt_ap, v_ap, qt_ap, qtok0):
                def sink_tok(ct):
                    k = {"f32": None, "b16": None, "T": False, "scale": 1.0}
                    if ct in OUTC and out_ap is not None:
                        k["f32"] = lambda tt: out_ap[tok0 + tt * 128: tok0 + (tt + 1) * 128, OUTC[ct]:OUTC[ct] + 512]
                    if ct in VC and v_ap is not None:
                        k["b16"] = lambda tt: v_ap[tok0 + tt * 128: tok0 + (tt + 1) * 128, VC[ct]:VC[ct] + 512]
                    if ct in KTH and kt_ap is not None:
                        k["T"] = True
                    if ct in QTH:
                        k["T"] = True
                        k["scale"] = QSCALE
                    return k

                def sink_T(ct, tsi):
                    if ct in KTH:
                        dst, h0, t0 = kt_ap, KTH[ct], tok0
                    else:
                        dst, h0, t0 = qt_ap, QTH[ct], qtok0
                    for j in range(4):
                        kb.dma(sp, dst[h0 + j, :, t0:t0 + ntok], tsi[:, j, 0:ntok], reads=[tsi.b], writes=[scrbuf])
                return sink_tok, sink_T

            jobs = []
            for g in range(S // TG):
                jobs.append((xf[g * TG:(g + 1) * TG, :], TG, [2, 3, 4, 5, 8, 9, 10, 11],
                             mk_sinks(g * TG, TG, kv_p, kt_scr, v_scr, None, 0)))
            for g in range(NOWN // TG):
                jobs.append((xo[g * TG:(g + 1) * TG, :], TG, [0, 1, 6, 7], mk_sinks(g * TG, TG, None, None, None, qt_scr, g * TG)))
            jobs.append((xs[:, :], NS, list(range(12)), mk_sinks(0, NS, kv_s, kts_scr, vs_scr, qts_scr, 0)))
            load_xT(jobs[0][0], jobs[0][1], xTs[0])
            for k, (src, ntok, cts, (s1, s2)) in enumerate(jobs):
                hook = None
                if k + 1 < len(jobs):
                    hook = (lambda k=k: load_xT(jobs[k + 1][0], jobs[k + 1][1], xTs[(k + 1) % 2]))
                proj_cols(ntok, cts, s1, s2, xTs[k % 2], hook)
        kb.barrier()

    if "proj" in cfg.stages:
        phase_proj()

    def phase_attn():
        with ExitStack() as st:
            VOFF = 20
            lam_t = sbt(kb, st, "lam_t", [128, 8], F32)
            prod = sbt(kb, st, "lprod", [128, 128], F32)
            gsc = sbt(kb, st, "gsc", [128, 2], F32)
            for k2 in range(2):
                dve.op(lambda e, k2=k2: e.tensor_tensor(out=prod[:], in0=vec_t[:, VOFF + 256 * k2: VOFF + 256 * k2 + 128],
                                                        in1=vec_t[:, VOFF + 256 * k2 + 128: VOFF + 256 * k2 + 256],
                                                        op=ALU.mult), reads=[vec_t.b], writes=[prod.b])
                dve.op(lambda e, k2=k2: e.reduce_sum(out=lam_t[:, k2:k2 + 1], in_=prod[:], axis=AX.X),
                       reads=[prod.b], writes=[lam_t.b])
            act.op(lambda e: e.activation(out=lam_t[:, 2:4], in_=lam_t[:, 0:2], func=AF.Exp), reads=[lam_t.b], writes=[lam_t.b])
            dve.op(lambda e: e.tensor_tensor(out=lam_t[:, 4:5], in0=lam_t[:, 3:4], in1=lam_t[:, 2:3], op=ALU.subtract),
                   reads=[lam_t.b], writes=[lam_t.b])
            dve.op(lambda e: e.tensor_scalar(out=lam_t[:, 5:6], in0=lam_t[:, 4:5], scalar1=-LAM_INIT, scalar2=None,
                                             op0=ALU.add), reads=[lam_t.b], writes=[lam_t.b])
            dve.op(lambda e: e.tensor_scalar(out=gsc[:], in0=vec_t[:, VOFF + 512: VOFF + 514], scalar1=1.0 - LAM_INIT,
                                             scalar2=None, op0=ALU.mult), reads=[vec_t.b], writes=[gsc.b])
            neglam = lam_t[:, 5:6]

            sbm = sbt(kb, st, "sbm", [128, 8, 512], BF16)
            dam = sbt(kb, st, "dam", [128, 4, 8, 512], BF16)
            sbm_s = sbt(kb, st, "sbm_s", [64, 512], BF16)
            dam_s = sbt(kb, st, "dam_s", [64, 512], BF16)
            at = sbt(kb, st, "at", [4, 72, 128], BF16)
            at_s = sbt(kb, st, "at_s", [4, NKS, 128], BF16)
            bt = sbt(kb, st, "bt", [4, 4, 512], BF16)
            bt_s = sbt(kb, st, "bt_s", [4, 512], BF16)
            for dst, src in ((sbm, sbmask), (dam, damask), (sbm_s, sbmask_s), (dam_s, damask_s), (at, atab),
                             (at_s, atab_s), (bt, btab), (bt_s, btab_s)):
                kb.dma(sp, dst[:], src, writes=[dst.b])

            ez = [sbt(kb, st, "ez%d" % i, [128, 512], F32) for i in range(2)]
            spt = [sbt(kb, st, "spt%d" % i, [128, 512], F32) for i in range(3)]
            l2 = [sbt(kb, st, "l2%d" % i, [128, 512], F32) for i in range(3)]
            Sc = [sbt(kb, st, "Sc%d" % i, [128, 512], F32) for i in range(2)]
            vt = [sbt(kb, st, "vt%d" % i, [128, 512], F32) for i in range(2)]
            et = [sbt(kb, st, "et%d" % i, [128, 512], BF16) for i in range(4)]
            ot = [sbt(kb, st, "ot%d" % i, [128, 512], BF16) for i in range(2)]
            dtmp = [sbt(kb, st, "dtmp%d" % i, [128, 512], F32) for i in range(5)]
            zero_f = sbt(kb, st, "zero_f", [128, 512], F32)
            pool.op(lambda e: e.memset(zero_f[:], 0.0), writes=[zero_f.b])
            ps = [pst(kb, st, "ps%d" % i, [128, 512], F32) for i in range(8)]
            cn = {"ot": 0}

            def sb_tile(groups, blocks, W, sink):
                pz, pc, po = ps[0:2], ps[2:4], ps[4 + (cn["ot"] % 2)]
                n = len(blocks)
                for j in range(2):
                    pool.op(lambda e, j=j: e.tensor_copy(out=Sc[j][:, 0:W].bitcast(F32R), in_=zero_f[:, 0:W]),
                            reads=[zero_f.b], writes=[Sc[j].b])

                def s_qk(i):
                    kbi, nk, mk = blocks[i]
                    z = pz[i % 2]
                    ng = len(groups)
                    for gi, (KT, Vg, QT, w) in enumerate(groups):
                        pe.op(lambda e, gi=gi, KT=KT, QT=QT, w=w: e.matmul(z[0:nk, gi * w:(gi + 1) * w], lhsT=KT(kbi, nk), rhs=QT, start=(gi == 0),
                                                                         stop=(mk is None and gi == ng - 1), skip_group_check=(ng > 1)),
                              reads=[ktb, qtb], writes=[z.b])
                    if mk is not None:
                        pe.op(lambda e: e.matmul(z[0:nk, 0:W], lhsT=ident_b[0:nk, 0:nk], rhs=mk, start=False, stop=True,
                                                 skip_group_check=(ng > 1)),
                              reads=[ident_b.b, sbm.b, sbm_s.b], writes=[z.b])

                def s_ez(i):
                    kbi, nk, mk = blocks[i]
                    z, a = pz[i % 2], ez[i % 2]
                    act.op(lambda e: e.activation(out=a[0:nk, 0:W], in_=z[0:nk, 0:W], func=AF.Exp), reads=[z.b], writes=[a.b])

                def s_sp(i):
                    kbi, nk, mk = blocks[i]
                    a, b_ = ez[i % 2], spt[i % 3]
                    act.op(lambda e: e.activation(out=b_[0:nk, 0:W].bitcast(F32R), in_=a[0:nk, 0:W], func=AF.Ln, bias=1.0),
                           reads=[a.b], writes=[b_.b])

                def s_l2(i):
                    kbi, nk, mk = blocks[i]
                    z, b_, c_ = pz[i % 2], spt[i % 3], l2[i % 3]
                    dve.op(lambda e: e.scalar_tensor_tensor(out=c_[0:nk, 0:W], in0=z[0:nk, 0:W], scalar=-1.0,
                                                            in1=b_[0:nk, 0:W], op0=ALU.mult, op1=ALU.add),
                           reads=[z.b, b_.b], writes=[c_.b])

                def s_cps(i):
                    kbi, nk, mk = blocks[i]
                    c = pc[i % 2]
                    b_ = spt[i % 3]
                    s_cur, s_nxt = Sc[i % 2], Sc[(i + 1) % 2]
                    if nk == 128:
                        pe.op(lambda e: e.matmul(c[0:nk, 0:W], lhsT=tri_r[:, :].bitcast(F32R), rhs=b_[0:nk, 0:W].bitcast(F32R),
                                                 start=True, stop=False), reads=[tri_r.b, b_.b], writes=[c.b])
                        pe.op(lambda e: e.matmul(c[0:nk, 0:W], lhsT=ones_r[:, :].bitcast(F32R), rhs=s_cur[0:128, 0:W].bitcast(F32R),
                                                 start=False, stop=True), reads=[ones_r.b, s_cur.b], writes=[c.b])
                    else:
                        pe.op(lambda e: e.matmul(c[0:nk, 0:W], lhsT=tri_f[0:nk, 0:nk], rhs=b_[0:nk, 0:W], start=True, stop=False),
                              reads=[tri_f.b, b_.b], writes=[c.b])
                        pe.op(lambda e: e.matmul(c[0:nk, 0:W], lhsT=ones_f[0:128, 0:nk], rhs=s_cur[0:128, 0:W], start=False, stop=True),
                              reads=[ones_f.b, s_cur.b], writes=[c.b])
                    if i + 1 < n:
                        pool.op(lambda e: e.tensor_tensor(out=s_nxt[0:nk, 0:W].bitcast(F32R), in0=s_cur[0:nk, 0:W], in1=b_[0:nk, 0:W],
                                                          op=ALU.add), reads=[s_cur.b, b_.b], writes=[s_nxt.b])

                def s_v(i):
                    kbi, nk, mk = blocks[i]
                    c, c_, v_ = pc[i % 2], l2[i % 3], vt[i % 2]
                    dve.op(lambda e: e.tensor_tensor(out=v_[0:nk, 0:W], in0=c[0:nk, 0:W], in1=c_[0:nk, 0:W], op=ALU.add),
                           reads=[c.b, c_.b], writes=[v_.b])

                def s_e(i):
                    kbi, nk, mk = blocks[i]
                    v_, e_ = vt[i % 2], et[i % 4]
                    act.op(lambda e: e.activation(out=e_[0:nk, 0:W], in_=v_[0:nk, 0:W], func=AF.Exp, scale=-1.0),
                           reads=[v_.b], writes=[e_.b])

                def s_pv(i):
                    kbi, nk, mk = blocks[i]
                    e_ = et[i % 4]
                    ng = len(groups)
                    for gi, (KT, Vg, QT, w) in enumerate(groups):
                        pe.op(lambda e, gi=gi, Vg=Vg, w=w: e.matmul(po[0:128, gi * w:(gi + 1) * w], lhsT=Vg(kbi, nk), rhs=e_[0:nk, gi * w:(gi + 1) * w],
                                                                  start=(i == 0 and gi == 0), stop=(i == n - 1), skip_group_check=(ng > 1)),
                              reads=[vb, e_.b], writes=[po.b])

                def ok(i):
                    return 0 <= i < n

                for t in range(n + 4):
                    if ok(t):
                        s_qk(t)
                    if ok(t - 1):
                        s_sp(t - 1)
                    if ok(t - 3):
                        s_v(t - 3)
                    if ok(t):
                        s_ez(t)
                    if ok(t - 1):
                        s_l2(t - 1)
                    if ok(t - 2):
                        s_cps(t - 2)
                    if ok(t - 3):
                        s_e(t - 3)
                    if ok(t - 4):
                        s_pv(t - 4)
                o_ = ot[cn["ot"] % 2]
                cn["ot"] += 1
                act.op(lambda e: e.copy(out=o_[:, 0:W], in_=po[:, 0:W]), reads=[po.b], writes=[o_.b])
                sink(o_)

            def da_tile(groups, blocks, Bap, W, sink0, sink1):
                pz = ps[0:2]
                pO = [[ps[2], ps[3]], [ps[4], ps[5]]]
                pZ = [ps[6], ps[7]]
                n = len(blocks)
                items = [(i, m) for i in range(n) for m in range(2)]

                def stA(q):
                    i, m = items[q]
                    kbi, nk, Aap, mk = blocks[i]
                    z = pz[q % 2]
                    ng = len(groups)
                    for gi, (KT0, KT1, Vg, QT0, QT1, w) in enumerate(groups):
                        pe.op(lambda e, gi=gi, KTm=(KT0, KT1)[m], QTm=(QT0, QT1)[m], w=w: e.matmul(
                            z[0:nk, gi * w:(gi + 1) * w], lhsT=KTm(kbi, nk), rhs=QTm, start=(gi == 0), stop=False, skip_group_check=(ng > 1)),
                              reads=[ktb, qtb], writes=[z.b])
                    pe.op(lambda e: e.matmul(z[0:nk, 0:W], lhsT=Aap, rhs=Bap, start=False, stop=(mk is None), skip_group_check=(ng > 1)),
                          reads=[at.b, at_s.b, bt.b, bt_s.b], writes=[z.b])
                    if mk is not None:
                        pe.op(lambda e: e.matmul(z[0:nk, 0:W], lhsT=ident_b[0:nk, 0:nk], rhs=mk, start=False, stop=True,
                                                 skip_group_check=(ng > 1)),
                              reads=[ident_b.b, dam.b, dam_s.b], writes=[z.b])
                    e_ = et[q % 4]
                    act.op(lambda e: e.activation(out=e_[0:nk, 0:W], in_=z[0:nk, 0:W], func=AF.Exp), reads=[z.b], writes=[e_.b])

                def stC(q):
                    i, m = items[q]
                    kbi, nk, Aap, mk = blocks[i]
                    e_ = et[q % 4]
                    ng = len(groups)
                    for c in range(2):
                        for gi, (KT0, KT1, Vg, QT0, QT1, w) in enumerate(groups):
                            pe.op(lambda e, c=c, gi=gi, Vg=Vg, w=w: e.matmul(pO[m][c][0:128, gi * w:(gi + 1) * w], lhsT=Vg(kbi, nk, c),
                                                                           rhs=e_[0:nk, gi * w:(gi + 1) * w], start=(i == 0 and gi == 0),
                                                                           stop=(i == n - 1), skip_group_check=(ng > 1)),
                                  reads=[vb, e_.b], writes=[pO[m][c].b])
                    pe.op(lambda e: e.matmul(pZ[m][0:128, 0:W], lhsT=ones_b[0:nk, 0:128], rhs=e_[0:nk, 0:W],
                                             start=(i == 0), stop=(i == n - 1)), reads=[ones_b.b, e_.b], writes=[pZ[m].b])

                for q in range(len(items) + 1):
                    if q < len(items):
                        stA(q)
                    if q - 1 >= 0:
                        stC(q - 1)
                rz = [ez[0], ez[1]]
                for m in range(2):
                    dve.op(lambda e, m=m: e.reciprocal(out=rz[m][:, 0:W], in_=pZ[m][:, 0:W]), reads=[pZ[m].b], writes=[rz[m].b])
                oc = [dtmp[0], dtmp[1]]
                sq = [dtmp[2], dtmp[3]]
                for c in range(2):
                    t0, t1 = vt[0], vt[1]
                    dve.op(lambda e, c=c: e.tensor_tensor(out=t0[:, 0:W], in0=pO[0][c][:, 0:W], in1=rz[0][:, 0:W], op=ALU.mult),
                           reads=[pO[0][c].b, rz[0].b], writes=[t0.b])
                    dve.op(lambda e, c=c: e.tensor_tensor(out=t1[:, 0:W], in0=pO[1][c][:, 0:W], in1=rz[1][:, 0:W], op=ALU.mult),
                           reads=[pO[1][c].b, rz[1].b], writes=[t1.b])
                    dve.op(lambda e, c=c: e.scalar_tensor_tensor(out=oc[c][:, 0:W], in0=t1[:, 0:W], scalar=neglam, in1=t0[:, 0:W],
                                                                 op0=ALU.mult, op1=ALU.add),
                           reads=[t0.b, t1.b, lam_t.b], writes=[oc[c].b])
                    act.op(lambda e, c=c: e.activation(out=sq[c][:, 0:W], in_=oc[c][:, 0:W], func=AF.Square),
                           reads=[oc[c].b], writes=[sq[c].b])
                pss = ps[0]
                for c in range(2):
                    pe.op(lambda e, c=c: e.matmul(pss[0:128, 0:W], lhsT=ones_f[:, :], rhs=sq[c][:, 0:W], start=(c == 0), stop=(c == 1)),
                          reads=[ones_f.b, sq[c].b], writes=[pss.b])
                rs = dtmp[4]
                act.op(lambda e: e.activation(out=rs[:, 0:W], in_=pss[:, 0:W], func=AF.Sqrt, scale=1.0 / 256.0, bias=RMS_EPS),
                       reads=[pss.b], writes=[rs.b])
                dve.op(lambda e: e.reciprocal(out=rs[:, 0:W], in_=rs[:, 0:W]), reads=[rs.b], writes=[rs.b])
                for c in range(2):
                    o_ = ot[cn["ot"] % 2]
                    cn["ot"] += 1
                    dve.op(lambda e, c=c, o_=o_: e.scalar_tensor_tensor(out=o_[:, 0:W], in0=oc[c][:, 0:W], scalar=gsc[:, c:c + 1],
                                                                        in1=rs[:, 0:W], op0=ALU.mult, op1=ALU.mult),
                           reads=[oc[c].b, gsc.b, rs.b], writes=[o_.b])
                    (sink0, sink1)[c](o_)

            ktb, qtb, vb = Buf("kt"), Buf("qt"), Buf("v")

            def mt_sink(mc, tok0, W):
                def f(o_):
                    kb.dma(sp, mt_scr[mc, :, tok0:tok0 + W], o_[:, 0:W], reads=[o_.b], writes=[scrbuf])
                return f

            def mt_sink_multi(mcs, tok0):
                def f(o_):
                    for gi, mc in enumerate(mcs):
                        kb.dma(sp, mt_scr[mc, :, tok0:tok0 + 64], o_[:, gi * 64:(gi + 1) * 64], reads=[o_.b], writes=[scrbuf])
                return f

            if "sb" in cfg.stages or "da" in cfg.stages:
                with ExitStack() as s2:
                    kt0 = sbt(kb, s2, "kt0", [128, S], BF16)
                    kt1 = sbt(kb, s2, "kt1", [128, S], BF16)
                    qt0 = sbt(kb, s2, "qt0", [128, NOWN], BF16)
                    qt1 = sbt(kb, s2, "qt1", [128, NOWN], BF16)
                    vh = sbt(kb, s2, "vh", [128, NKBP, 256], BF16)
                    if "sb" in cfg.stages:
                        for h in range(8):
                            kb.dma(sp, kt0[:], kt_scr[h], reads=[scrbuf], writes=[ktb])
                            kb.dma(sp, qt0[:], qt_scr[h], reads=[scrbuf], writes=[qtb])
                            for k0 in range(0, NKBP, 16):
                                kb.dma(sp, vh[:, k0:k0 + 16, 0:128],
                                       v_scr[k0 * 128:(k0 + 16) * 128, h * 128:(h + 1) * 128].rearrange("(k p) d -> p k d", p=128),
                                       reads=[scrbuf], writes=[vb])
                            for l in range(NL):
                                blocks = []
                                for kbi in range(8 * l + 7, -1, -1):
                                    mk = sbm[:, kbi - 8 * l, :] if kbi >= 8 * l else None
                                    blocks.append((kbi, 128, mk))
                                sb_tile([(lambda kbi, nk: kt0[:, kbi * 128: kbi * 128 + nk],
                                          lambda kbi, nk: vh[0:nk, kbi, 0:128],
                                          qt0[:, l * 512:(l + 1) * 512], 512)], blocks, 512, mt_sink(h, l * 512, 512))
                    if "da" in cfg.stages:
                        for h in range(4):
                            kb.dma(sp, kt0[:], kt_scr[8 + 2 * h], reads=[scrbuf], writes=[ktb])
                            kb.dma(sp, kt1[:], kt_scr[9 + 2 * h], reads=[scrbuf], writes=[ktb])
                            kb.dma(sp, qt0[:], qt_scr[8 + 2 * h], reads=[scrbuf], writes=[qtb])
                            kb.dma(sp, qt1[:], qt_scr[9 + 2 * h], reads=[scrbuf], writes=[qtb])
                            for k0 in range(0, NKBP, 16):
                                kb.dma(sp, vh[:, k0:k0 + 16, :],
                                       v_scr[k0 * 128:(k0 + 16) * 128, 1024 + h * 256: 1024 + (h + 1) * 256].rearrange("(k p) d -> p k d", p=128),
                                       reads=[scrbuf], writes=[vb])
                            for l in range(NL):
                                blocks = []
                                for kbi in range(8 * l + 8):
                                    mk = dam[:, h, kbi - 8 * l, :] if kbi >= 8 * l else None
                                    blocks.append((kbi, 128, at[:, kbi - 8 * l + 64, :], mk))
                                da_tile([(lambda kbi, nk: kt0[:, kbi * 128: kbi * 128 + nk],
                                          lambda kbi, nk: kt1[:, kbi * 128: kbi * 128 + nk],
                                          lambda kbi, nk, c: vh[0:nk, kbi, c * 128:(c + 1) * 128],
                                          qt0[:, l * 512:(l + 1) * 512], qt1[:, l * 512:(l + 1) * 512], 512)], blocks, bt[:, h, :], 512,
                                        mt_sink(8 + 2 * h, l * 512, 512), mt_sink(9 + 2 * h, l * 512, 512))

            kb.barrier()
            with ExitStack() as s2:
                NKC = PAST // 128
                kts = sbt(kb, s2, "kts", [128, 4, PAST + 64], BF16)
                vs = sbt(kb, s2, "vs", [128, NKC + 1, 512], BF16)
                qts = sbt(kb, s2, "qts", [128, 4, 64], BF16)
                cb = [sbt(kb, s2, "cb%d" % i, [128, 512], BF16) for i in range(2)]
                ptr = ps[7]
                ncb = {"i": 0}

                def prep(b, ck, cv, half, kt_idx0, v_col0):
                    for kbi in range(NKC):
                        c_ = cb[ncb["i"] % 2]
                        ncb["i"] += 1
                        kb.dma(pool, c_[:], ck[b, kbi * 128:(kbi + 1) * 128, half * 512:(half + 1) * 512], writes=[c_.b])
                        pt = ptr
                        for j in range(4):
                            pe.op(lambda e, j=j: e.matmul(pt[:, j * 128:(j + 1) * 128], lhsT=c_[:, j * 128:(j + 1) * 128],
                                                          rhs=ident_b[:, :], start=True, stop=True),
                                  reads=[c_.b, ident_b.b], writes=[pt.b])
                        dve.op(lambda e, kbi=kbi: e.tensor_copy(out=kts[:, :, kbi * 128:(kbi + 1) * 128],
                                                                in_=pt[:, :].rearrange("p (h t) -> p h t", h=4)),
                               reads=[pt.b], writes=[ktb])
                    for k0 in range(0, NKC, 8):
                        k1 = min(NKC, k0 + 8)
                        kb.dma(pool, vs[:, k0:k1, :],
                               cv[b, k0 * 128:k1 * 128, half * 512:(half + 1) * 512].rearrange("(k p) d -> p k d", p=128),
                               writes=[vb])
                    for j in range(4):
                        kb.dma(sp, kts[:, j, PAST:PAST + 64], kts_scr[kt_idx0 + j, :, b * 64:(b + 1) * 64], reads=[scrbuf], writes=[ktb])
                        kb.dma(sp, qts[:, j, :], qts_scr[kt_idx0 + j, :, b * 64:(b + 1) * 64], reads=[scrbuf], writes=[qtb])
                    kb.dma(sp, vs[0:64, NKC, :], vs_scr[b * 64:(b + 1) * 64, v_col0:v_col0 + 512], reads=[scrbuf], writes=[vb])

                for b in range(NSB):
                    tok0 = NOWN + b * 64
                    if "sb" in cfg.stages:
                        for half in range(2):
                            prep(b, csk, csv, half, half * 4, half * 512)
                            blocks = [(NKC, 64, sbm_s[:, 0:256])] + [(kbi, 128, None) for kbi in range(NKC - 1, -1, -1)]
                            groups = [(lambda kbi, nk, j=j: kts[:, j, kbi * 128: kbi * 128 + nk],
                                       lambda kbi, nk, j=j: vs[0:nk, kbi, j * 128:(j + 1) * 128],
                                       qts[:, j, :], 64) for j in range(4)]
                            sb_tile(groups, blocks, 256, mt_sink_multi([half * 4 + j for j in range(4)], tok0))
                    if "da" in cfg.stages:
                        for half in range(2):
                            prep(b, cdk, cdv, half, 8 + half * 4, 1024 + half * 512)
                            h0 = half * 2
                            blocks = [(kbi, 128, at_s[:, kbi, :], None) for kbi in range(NKC)]
                            blocks.append((NKC, 64, at_s[:, NKC, 0:64], dam_s[:, h0 * 64:(h0 + 2) * 64]))
                            groups = [(lambda kbi, nk, jj=jj: kts[:, 2 * jj, kbi * 128: kbi * 128 + nk],
                                       lambda kbi, nk, jj=jj: kts[:, 2 * jj + 1, kbi * 128: kbi * 128 + nk],
                                       lambda kbi, nk, c, jj=jj: vs[0:nk, kbi, jj * 256 + c * 128: jj * 256 + (c + 1) * 128],
                                       qts[:, 2 * jj, :], qts[:, 2 * jj + 1, :], 64) for jj in range(2)]
                            da_tile(groups, blocks, bt_s[:, h0 * 64:(h0 + 2) * 64], 128,
                                    mt_sink_multi([8 + 2 * (h0 + jj) for jj in range(2)], tok0),
                                    mt_sink_multi([9 + 2 * (h0 + jj) for jj in range(2)], tok0))
        kb.barrier()

    if "sb" in cfg.stages or "da" in cfg.stages:
        phase_attn()


    def phase_ffn():
        with ExitStack() as st:
            G = 512
            lnt = sbt(kb, st, "lnt", [128, 4 * D], F32)
            kb.dma(sp, lnt[:], lnv[:, :], writes=[lnt.b])
            wr_t = sbt(kb, st, "wr_t", [128, 16, 20], F32)
            kb.dma(sp, wr_t[:], w_r.rearrange("(c p) n -> p c n", p=128), writes=[wr_t.b])
            sel_t = sbt(kb, st, "sel_t", [16, 16, 128], F32)
            kb.dma(sp, sel_t[:], sel[:, :, :], writes=[sel_t.b])
            W = [sbt(kb, st, "W%d" % i, [128, 8192], BF16) for i in range(5)]
            mtile = sbt(kb, st, "mtile", [128, 16, G], BF16)
            u = [sbt(kb, st, "u%d" % i, [128, D], F32) for i in range(4)]
            h1Tf = sbt(kb, st, "h1Tf", [128, 16, 128], F32)
            hT = [sbt(kb, st, "hT%d" % i, [128, 4, G], BF16) for i in range(2)]
            sg = [sbt(kb, st, "sg%d" % i, [128, G], F32) for i in range(2)]
            hu = [sbt(kb, st, "hu%d" % i, [128, G], F32) for i in range(2)]
            lg = sbt(kb, st, "lg", [128, 20], F32)
            g1 = sbt(kb, st, "g1", [128, 16], F32)
            oh = sbt(kb, st, "oh", [128, 4], F32)
            fs = sbt(kb, st, "fs", [128, 4], F32)
            fs2 = sbt(kb, st, "fs2", [128, 4], F32)
            eq1 = sbt(kb, st, "eq1", [128, 4], F32)
            eq2 = sbt(kb, st, "eq2", [128, 4], F32)
            wi = sbt(kb, st, "wi", [128, 4], F32)
            junk = sbt(kb, st, "junk", [128, 4], F32)
            comb = sbt(kb, st, "comb", [128, 16], F32)
            combT = sbt(kb, st, "combT", [16, G], F32)
            stats = sbt(kb, st, "stats", [128, 4, 6], F32)
            mv = sbt(kb, st, "mv", [128, 4], F32)
            ps = [pst(kb, st, "pf%d" % i, [128, 512], F32) for i in range(8)]
            pg, pu, pcb, py, ptr = ps[0:2], ps[2:4], ps[4], ps[5:7], ps[7]
            cn = {"w": 0, "acc": 0, "y": 0, "gu": 0}

            def wnext():
                w = W[cn["w"] % 5]
                cn["w"] += 1
                return w

            def layer_norm(ut, goff):
                for k4 in range(4):
                    dve.op(lambda e, k4=k4: e.bn_stats(out=stats[:, k4, :], in_=ut[:, k4 * 512:(k4 + 1) * 512]),
                           reads=[ut.b], writes=[stats.b])
                dve.op(lambda e: e.bn_aggr(out=mv[:, 0:2], in_=stats[:].rearrange("p a b -> p (a b)")),
                       reads=[stats.b], writes=[mv.b])
                act.op(lambda e: e.activation(out=mv[:, 2:3], in_=mv[:, 1:2], func=AF.Sqrt, bias=LN_EPS, scale=1.0),
                       reads=[mv.b], writes=[mv.b])
                dve.op(lambda e: e.reciprocal(out=mv[:, 3:4], in_=mv[:, 2:3]), reads=[mv.b], writes=[mv.b])
                dve.op(lambda e: e.tensor_scalar(out=ut[:], in0=ut[:], scalar1=mv[:, 0:1], scalar2=mv[:, 3:4],
                                                 op0=ALU.subtract, op1=ALU.mult), reads=[ut.b, mv.b], writes=[ut.b])
                pool.op(lambda e: e.tensor_tensor(out=ut[:], in0=ut[:], in1=lnt[:, goff:goff + D], op=ALU.mult),
                        reads=[ut.b, lnt.b], writes=[ut.b])
                dve.op(lambda e: e.tensor_tensor(out=ut[:], in0=ut[:], in1=lnt[:, goff + D:goff + 2 * D], op=ALU.add),
                       reads=[ut.b, lnt.b], writes=[ut.b])

            def gating(tt):
                pr = ptr
                for c in range(16):
                    pe.op(lambda e, c=c: e.matmul(pr[:, 0:20], lhsT=h1Tf[:, c, :], rhs=wr_t[:, c, :], start=(c == 0), stop=(c == 15)),
                          reads=[h1Tf.b, wr_t.b], writes=[pr.b])
                R_, Wr = [lg.b, g1.b, oh.b, fs.b, fs2.b, eq1.b, eq2.b, wi.b, comb.b], None
                dve.op(lambda e: e.tensor_tensor(out=lg[:], in0=pr[:, 0:20], in1=vec_t[:, 0:20], op=ALU.add),
                       reads=[pr.b, vec_t.b], writes=[lg.b])
                dve.op(lambda e: e.reduce_max(out=g1[:, 0:1], in_=lg[:, 0:4], axis=AX.X), reads=[lg.b], writes=[g1.b])
                dve.op(lambda e: e.tensor_scalar(out=oh[:], in0=lg[:, 0:4], scalar1=g1[:, 0:1], scalar2=None, op0=ALU.is_equal),
                       reads=[lg.b, g1.b], writes=[oh.b])
                dve.op(lambda e: e.tensor_scalar(out=g1[:, 1:2], in0=g1[:, 0:1], scalar1=-1.0, scalar2=None, op0=ALU.mult),
                       reads=[g1.b], writes=[g1.b])
                act.op(lambda e: e.activation(out=junk[:], in_=lg[:, 0:4], func=AF.Exp, bias=g1[:, 1:2], scale=1.0,
                                              accum_out=g1[:, 2:3]), reads=[lg.b, g1.b], writes=[junk.b, g1.b])
                dve.op(lambda e: e.reciprocal(out=g1[:, 3:4], in_=g1[:, 2:3]), reads=[g1.b], writes=[g1.b])
                for g in range(4):
                    if g == 0:
                        dve.op(lambda e: e.tensor_scalar(out=fs[:], in0=lg[:, 4:8], scalar1=oh[:, 0:1], scalar2=None, op0=ALU.mult),
                               reads=[lg.b, oh.b], writes=[fs.b])
                    else:
                        dve.op(lambda e, g=g: e.scalar_tensor_tensor(out=fs[:], in0=lg[:, 4 + 4 * g: 8 + 4 * g], scalar=oh[:, g:g + 1],
                                                                     in1=fs[:], op0=ALU.mult, op1=ALU.add),
                               reads=[lg.b, oh.b, fs.b], writes=[fs.b])
                dve.op(lambda e: e.reduce_max(out=g1[:, 4:5], in_=fs[:], axis=AX.X), reads=[fs.b], writes=[g1.b])
                dve.op(lambda e: e.tensor_scalar(out=eq1[:], in0=fs[:], scalar1=g1[:, 4:5], scalar2=None, op0=ALU.is_equal),
                       reads=[fs.b, g1.b], writes=[eq1.b])
                dve.op(lambda e: e.scalar_tensor_tensor(out=fs2[:], in0=eq1[:], scalar=-1e30, in1=fs[:], op0=ALU.mult, op1=ALU.add),
                       reads=[eq1.b, fs.b], writes=[fs2.b])
                dve.op(lambda e: e.reduce_max(out=g1[:, 5:6], in_=fs2[:], axis=AX.X), reads=[fs2.b], writes=[g1.b])
                dve.op(lambda e: e.tensor_scalar(out=eq2[:], in0=fs2[:], scalar1=g1[:, 5:6], scalar2=None, op0=ALU.is_equal),
                       reads=[fs2.b, g1.b], writes=[eq2.b])
                dve.op(lambda e: e.tensor_tensor(out=g1[:, 6:7], in0=g1[:, 5:6], in1=g1[:, 4:5], op=ALU.subtract),
                       reads=[g1.b], writes=[g1.b])
                act.op(lambda e: e.activation(out=g1[:, 7:8], in_=g1[:, 6:7], func=AF.Exp), reads=[g1.b], writes=[g1.b])
                dve.op(lambda e: e.tensor_scalar(out=g1[:, 8:9], in0=g1[:, 7:8], scalar1=1.0, scalar2=None, op0=ALU.add),
                       reads=[g1.b], writes=[g1.b])
                dve.op(lambda e: e.reciprocal(out=g1[:, 9:10], in_=g1[:, 8:9]), reads=[g1.b], writes=[g1.b])
                dve.op(lambda e: e.tensor_tensor(out=g1[:, 10:11], in0=g1[:, 9:10], in1=g1[:, 3:4], op=ALU.mult),
                       reads=[g1.b], writes=[g1.b])
                dve.op(lambda e: e.tensor_tensor(out=g1[:, 11:12], in0=g1[:, 7:8], in1=g1[:, 10:11], op=ALU.mult),
                       reads=[g1.b], writes=[g1.b])
                dve.op(lambda e: e.tensor_scalar(out=wi[:], in0=eq1[:], scalar1=g1[:, 10:11], scalar2=None, op0=ALU.mult),
                       reads=[eq1.b, g1.b], writes=[wi.b])
                dve.op(lambda e: e.scalar_tensor_tensor(out=wi[:], in0=eq2[:], scalar=g1[:, 11:12], in1=wi[:], op0=ALU.mult, op1=ALU.add),
                       reads=[eq2.b, g1.b, wi.b], writes=[wi.b])
                for g in range(4):
                    dve.op(lambda e, g=g: e.tensor_scalar(out=comb[:, 4 * g:4 * g + 4], in0=wi[:], scalar1=oh[:, g:g + 1], scalar2=None,
                                                          op0=ALU.mult), reads=[wi.b, oh.b], writes=[comb.b])
                pe.op(lambda e: e.matmul(pr[0:16, 128:256], lhsT=comb[:, 0:16], rhs=ident_f[:, :], start=True, stop=True),
                      reads=[comb.b, ident_f.b], writes=[pr.b])
                act.op(lambda e: e.copy(out=combT[0:16, tt * 128:(tt + 1) * 128], in_=pr[0:16, 128:256]), reads=[pr.b], writes=[combT.b])

            ngroups = (NTOK + G - 1) // G
            for gi in range(ngroups):
                tok0 = gi * G
                Gc = min(G, NTOK - tok0)
                ntt = Gc // 128
                kb.dma(sp, mtile[:, :, 0:Gc], mt_scr[:, :, tok0:tok0 + Gc].rearrange("c p t -> p c t"), reads=[scrbuf], writes=[mtile.b])
                for tt in range(ntt):
                    tk = tok0 + tt * 128
                    src = xo[tk:tk + 128, :] if tk < NOWN else xs[tk - NOWN: tk - NOWN + 128, :]
                    kb.dma(sp, u[tt][:], src, writes=[u[tt].b])
                for ct in range(4):
                    w = wnext()
                    wv = w.t[:, :].rearrange("p (c n) -> p c n", c=16)
                    kb.dma(pool, wv, w_out.rearrange("(c p) n -> p c n", p=128)[:, :, ct * 512:(ct + 1) * 512], writes=[w.b])
                    for tt in range(ntt):
                        pa = py[cn["y"] % 2]
                        cn["y"] += 1
                        for c in range(16):
                            pe.op(lambda e, c=c, tt=tt, pa=pa: e.matmul(pa[:], lhsT=mtile[:, c, tt * 128:(tt + 1) * 128], rhs=wv[:, c, :],
                                                                         start=(c == 0), stop=(c == 15)),
                                  reads=[mtile.b, w.b], writes=[pa.b])
                        dve.op(lambda e, tt=tt, ct=ct, pa=pa: e.scalar_tensor_tensor(
                            out=u[tt][:, ct * 512:(ct + 1) * 512], in0=u[tt][:, ct * 512:(ct + 1) * 512], scalar=ALPHA,
                            in1=pa[:], op0=ALU.mult, op1=ALU.add), reads=[u[tt].b, pa.b], writes=[u[tt].b])
                h1Tb = mtile
                for tt in range(ntt):
                    layer_norm(u[tt], 0)
                    for q4 in range(4):
                        for j in range(4):
                            c = q4 * 4 + j
                            pe.op(lambda e, c=c, j=j, tt=tt: e.transpose(ptr[:, j * 128:(j + 1) * 128], u[tt][:, c * 128:(c + 1) * 128], ident_f[:]),
                                  reads=[u[tt].b, ident_f.b], writes=[ptr.b])
                        act.op(lambda e, q4=q4: e.copy(out=h1Tf[:, q4 * 4:(q4 + 1) * 4, :], in_=ptr[:].rearrange("p (c t) -> p c t", c=4)),
                               reads=[ptr.b], writes=[h1Tf.b])
                    pool.op(lambda e, tt=tt: e.tensor_copy(out=h1Tb[:, :, tt * 128:(tt + 1) * 128], in_=h1Tf[:]),
                            reads=[h1Tf.b], writes=[h1Tb.b])
                    gating(tt)
                    dve.op(lambda e, tt=tt: e.tensor_scalar(out=u[tt][:], in0=u[tt][:], scalar1=ALPHA, scalar2=None, op0=ALU.mult),
                           reads=[u[tt].b], writes=[u[tt].b])
                for ex in range(16):
                    wg_, wu_, wd_ = wnext(), wnext(), wnext()
                    wgv = wg_.t[:, :].rearrange("p (c n) -> p c n", c=16)
                    wuv = wu_.t[:, :].rearrange("p (c n) -> p c n", c=16)
                    wdv = wd_.t[:, :].rearrange("p (c n) -> p c n", c=4)
                    kb.dma(pool, wgv, w_gate[ex].rearrange("(c p) n -> p c n", p=128), writes=[wg_.b])
                    kb.dma(pool, wuv, w_up[ex].rearrange("(c p) n -> p c n", p=128), writes=[wu_.b])
                    for hh in range(2):
                        kb.dma(pool, wdv[:, :, hh * 1024:(hh + 1) * 1024],
                               w_down[ex].rearrange("(c p) n -> p c n", p=128)[:, :, hh * 1024:(hh + 1) * 1024], writes=[wd_.b])
                    pe.op(lambda e, ex=ex: e.matmul(pcb[:, 0:Gc], lhsT=sel_t[0:16, ex, :], rhs=combT[0:16, 0:Gc], start=True, stop=True),
                          reads=[sel_t.b, combT.b], writes=[pcb.b])
                    hTe = hT[ex % 2]
                    for fc in range(4):
                        i = cn["gu"]
                        cn["gu"] += 1
                        pg_, pu_ = pg[i % 2], pu[i % 2]
                        for c in range(16):
                            pe.op(lambda e, c=c, fc=fc, pg_=pg_: e.matmul(pg_[:, 0:Gc], lhsT=wgv[:, c, fc * 128:(fc + 1) * 128], rhs=h1Tb[:, c, 0:Gc],
                                                                           start=(c == 0), stop=(c == 15)), reads=[wg_.b, h1Tb.b], writes=[pg_.b])
                        for c in range(16):
                            pe.op(lambda e, c=c, fc=fc, pu_=pu_: e.matmul(pu_[:, 0:Gc], lhsT=wuv[:, c, fc * 128:(fc + 1) * 128], rhs=h1Tb[:, c, 0:Gc],
                                                                           start=(c == 0), stop=(c == 15)), reads=[wu_.b, h1Tb.b], writes=[pu_.b])
                        sg_, hu_ = sg[i % 2], hu[i % 2]
                        act.op(lambda e, sg_=sg_, pg_=pg_: e.activation(out=sg_[:, 0:Gc], in_=pg_[:, 0:Gc], func=AF.Silu),
                               reads=[pg_.b], writes=[sg_.b])
                        dve.op(lambda e, sg_=sg_, pu_=pu_, hu_=hu_: e.tensor_tensor(out=hu_[:, 0:Gc], in0=sg_[:, 0:Gc], in1=pu_[:, 0:Gc], op=ALU.mult),
                               reads=[sg_.b, pu_.b], writes=[hu_.b])
                        dve.op(lambda e, hu_=hu_, fc=fc, hTe=hTe: e.tensor_tensor(out=hTe[:, fc, 0:Gc], in0=hu_[:, 0:Gc], in1=pcb[:, 0:Gc], op=ALU.mult),
                               reads=[hu_.b, pcb.b], writes=[hTe.b])
                    for tt in range(ntt):
                        for ct in range(4):
                            pa = py[cn["y"] % 2]
                            cn["y"] += 1
                            for fc in range(4):
                                pe.op(lambda e, fc=fc, tt=tt, ct=ct, pa=pa, hTe=hTe: e.matmul(
                                    pa[:], lhsT=hTe[:, fc, tt * 128:(tt + 1) * 128], rhs=wdv[:, fc, ct * 512:(ct + 1) * 512],
                                    start=(fc == 0), stop=(fc == 3)), reads=[hTe.b, wd_.b], writes=[pa.b])
                            dve.op(lambda e, tt=tt, ct=ct, pa=pa: e.tensor_tensor(out=u[tt][:, ct * 512:(ct + 1) * 512],
                                                                                  in0=u[tt][:, ct * 512:(ct + 1) * 512], in1=pa[:], op=ALU.add),
                                   reads=[u[tt].b, pa.b], writes=[u[tt].b])
                for tt in range(ntt):
                    layer_norm(u[tt], 2 * D)
                    kb.dma(sp, y_out[tok0 + tt * 128: tok0 + (tt + 1) * 128, :], u[tt][:], reads=[u[tt].b], writes=[scrbuf])
        kb.barrier()

    if "ffn" in cfg.stages:
        phase_ffn()

    kb.barrier()
    es.close()
    return nc


def _tables(cfg, hq):
    bf = ml_dtypes.bfloat16
    p = np.arange(128)[:, None]
    f = np.arange(512)[None, :]
    tabs = np.zeros((128, 512), np.float32)
    tabs[:, 0:128] = np.eye(128, dtype=np.float32)
    jj = np.arange(128)[:, None]
    ss = np.arange(128)[None, :]
    tabs[:, 128:256] = (jj > ss).astype(np.float32)
    tabs[:, 256:384] = 1.0
    sbm = np.zeros((128, 8, 512), np.float32)
    dam = np.zeros((128, 4, 8, 512), np.float32)
    for r in range(8):
        s = 128 * r + p
        t = 512 * hq + f
        sbm[:, r, :] = np.where(s < t, 0.0, NEGM)
        vis = (s // 64) <= (t // 64)
        for h in range(4):
            corr = np.where(s > t, -2.0 * SLOPES[h] * (s - t), 0.0)
            dam[:, h, r, :] = np.where(vis, corr, NEGM)
    p64 = np.arange(64)[:, None]
    i64 = np.arange(512)[None, :] % 64
    sbm_s = np.where(p64 < i64, 0.0, NEGM).astype(np.float32)
    hcol = (np.arange(512) // 64 % 4)[None, :]
    slope_col = np.array(SLOPES, np.float32)[hcol]
    dam_s = np.where(p64 > i64, -2.0 * slope_col * (p64 - i64), 0.0).astype(np.float32)
    atab = np.zeros((4, 72, 128), np.float32)
    for j in range(72):
        rb = j - 64
        atab[0, j, :] = 1.0
        atab[1, j, :] = 1.0
        atab[2, j, :] = rb - 4 * hq
        atab[3, j, :] = np.arange(128)
    nks = cfg.PAST // 128 + 1
    atab_s = np.zeros((4, nks, 128), np.float32)
    for kbi in range(nks):
        atab_s[0, kbi, :] = 1.0
        atab_s[1, kbi, :] = 1.0
        atab_s[2, kbi, :] = kbi - (nks - 1)
        atab_s[3, kbi, :] = np.arange(128)
    btab = np.zeros((4, 4, 512), np.float32)
    tt = np.arange(512)
    for h in range(4):
        btab[0, h, :] = -SLOPES[h] * 128.0 * (tt // 128)
        btab[1, h, :] = -SLOPES[h] * (tt % 128)
        btab[2, h, :] = SLOPES[h] * 128.0
        btab[3, h, :] = SLOPES[h]
    btab_s = np.zeros((4, 512), np.float32)
    ii = np.arange(512) % 64
    sc = slope_col[0]
    btab_s[0, :] = 0.0
    btab_s[1, :] = -sc * ii
    btab_s[2, :] = sc * 128.0
    btab_s[3, :] = sc
    return dict(tabs=tabs, sbmask=sbm.astype(bf), damask=dam.astype(bf), sbmask_s=sbm_s.astype(bf),
                damask_s=dam_s.astype(bf), atab=atab.astype(bf), atab_s=atab_s.astype(bf),
                btab=btab.astype(bf), btab_s=btab_s.astype(bf))


def make_in_maps(cfg, inp):
    f32 = np.float32
    S, NSB = cfg.S, cfg.NSB
    x_prompt = np.asarray(inp["x_prompt"], f32)
    x_sample = np.asarray(inp["x_sample"], f32)
    w_r = np.ascontiguousarray(np.concatenate(
        [np.asarray(inp["w_coarse"], f32)[0], np.asarray(inp["w_fine"], f32)[0].reshape(D, 16)], axis=1))
    b_r = np.concatenate([np.asarray(inp["b_coarse"], f32)[0], np.asarray(inp["b_fine"], f32)[0].reshape(16)])
    sub = np.asarray(inp["subln_g"], f32)[0]
    vec_row = np.concatenate([b_r,
                              np.asarray(inp["lambda_q1"], f32)[0], np.asarray(inp["lambda_k1"], f32)[0],
                              np.asarray(inp["lambda_q2"], f32)[0], np.asarray(inp["lambda_k2"], f32)[0]])
    lnrow = np.concatenate([np.asarray(inp["ln1_g"], f32)[0], np.asarray(inp["ln1_b"], f32)[0],
                            np.asarray(inp["ln2_g"], f32)[0], np.asarray(inp["ln2_b"], f32)[0]])
    lnv = np.ascontiguousarray(np.broadcast_to(lnrow[None, :], (128, 4 * D)))
    vecs = np.zeros((128, 20 + 4 * 128 + 2), f32)
    vecs[:, :vec_row.size] = vec_row[None, :]
    vecs[:, vec_row.size] = sub[0:128]
    vecs[:, vec_row.size + 1] = sub[128:256]
    common = dict(w_in=np.ascontiguousarray(np.asarray(inp["w_in"], f32)[0]), vecs=vecs)
    if "ffn" in cfg.stages:
        common.update(
            w_out=np.ascontiguousarray(np.asarray(inp["w_out"], f32)[0]),
            w_gate=np.ascontiguousarray(np.asarray(inp["w_gate"], f32)[0]),
            w_up=np.ascontiguousarray(np.asarray(inp["w_up"], f32)[0]),
            w_down=np.ascontiguousarray(np.asarray(inp["w_down"], f32)[0]),
            w_r=w_r, lnv=lnv, sel=np.ascontiguousarray(np.broadcast_to(np.eye(16, dtype=f32)[:, :, None], (16, 16, 128))))
    maps = []
    for c in range(8):
        b, hq = c // 2, c % 2
        xfull = np.ascontiguousarray(x_prompt[b])
        xown = np.ascontiguousarray(xfull.reshape(S // 512, 512, D)[hq::2].reshape(S // 2, D))
        sl = slice(c * NSB, (c + 1) * NSB)
        m = dict(common)
        m.update(xf=xfull, xo=xown, xs=np.ascontiguousarray(x_sample[sl].reshape(NSB * 64, D)),
                 csk=np.ascontiguousarray(np.asarray(inp["cache_sb_k"], f32)[0, sl].reshape(NSB, cfg.PAST, 1024)),
                 csv=np.ascontiguousarray(np.asarray(inp["cache_sb_v"], f32)[0, sl].reshape(NSB, cfg.PAST, 1024)),
                 cdk=np.ascontiguousarray(np.asarray(inp["cache_da_k"], f32)[0, sl].reshape(NSB, cfg.PAST, 1024)),
                 cdv=np.ascontiguousarray(np.asarray(inp["cache_da_v"], f32)[0, sl].reshape(NSB, cfg.PAST, 1024)))
        m.update(_tables(cfg, hq))
        maps.append(m)
    return maps


def assemble(cfg, results):
    S, NSB = cfg.S, cfg.NSB
    NB = 4
    y_p = np.zeros((NB, S, D), np.float32)
    y_s = np.zeros((8 * NSB, 64, D), np.float32)
    kvp = np.zeros((NB, S, 4096), np.float32)
    kvs = np.zeros((8 * NSB, 64, 4096), np.float32)
    for c in range(8):
        b, hq = c // 2, c % 2
        r = results[c]
        yo = r["y_out"]
        y_p[b].reshape(S // 512, 512, D)[hq::2] = yo[:S // 2].reshape(S // 1024, 512, D)
        y_s[c * NSB:(c + 1) * NSB] = yo[S // 2:].reshape(NSB, 64, D)
        half = slice(hq * (S // 2), (hq + 1) * (S // 2))
        kvp[b, half] = r["kv_p"][half]
        kvs[c * NSB:(c + 1) * NSB] = r["kv_s"].reshape(NSB, 64, 4096)
    outs = (y_p, y_s,
            kvp[None, :, :, 0:1024].reshape(1, NB, S, 8, 128), kvp[None, :, :, 1024:2048].reshape(1, NB, S, 8, 128),
            kvp[None, :, :, 2048:3072].reshape(1, NB, S, 4, 2, 128), kvp[None, :, :, 3072:4096].reshape(1, NB, S, 4, 256),
            kvs[None, :, :, 0:1024].reshape(1, 8 * NSB, 64, 8, 128), kvs[None, :, :, 1024:2048].reshape(1, 8 * NSB, 64, 8, 128),
            kvs[None, :, :, 2048:3072].reshape(1, 8 * NSB, 64, 4, 2, 128), kvs[None, :, :, 3072:4096].reshape(1, 8 * NSB, 64, 4, 256))
    return tuple(np.ascontiguousarray(o) for o in outs)


def run(cfg, inp):
    nc = build(cfg)
    maps = make_in_maps(cfg, inp)
    res = run_bass_kernel_spmd(nc, maps, core_ids=list(range(8)))
    return assemble(cfg, res.results), res


def kernel(**inputs):
    cfg = Cfg()
    outs, _ = run(cfg, inputs)
    return outs
```

```python
from contextlib import ExitStack
import numpy as np
import ml_dtypes
import concourse.bass as bass
import concourse.mybir as mybir
from concourse.bass_utils import run_bass_kernel_spmd

F32 = mybir.dt.float32
F32R = mybir.dt.float32r
BF16 = mybir.dt.bfloat16
AF = mybir.ActivationFunctionType
ALU = mybir.AluOpType
AX = mybir.AxisListType

D = 2048
DIN = 6144
NEGM = -30000.0
ALPHA = 2.0 ** 0.25
LN_EPS = 1e-5
RMS_EPS = 1e-5
LAM_INIT = 0.8 - 0.6
SLOPES = [2.0 ** (-8.0 * (h + 1) / 4) for h in range(4)]
QSCALE = 128.0 ** -0.5


class Cfg:
    def __init__(self, S=8192, PAST=4096, NSB=4, stages=("proj", "sb", "da", "ffn"), debug=False):
        self.S = S
        self.PAST = PAST
        self.NSB = NSB
        self.DS = 64
        self.NL = S // 1024
        self.NOWN = S // 2
        self.NTOK = self.NOWN + NSB * 64
        self.stages = stages
        self.debug = debug


class Buf:
    __slots__ = ("name", "w", "r")

    def __init__(self, name=""):
        self.name = name
        self.w = {}
        self.r = {}


class Eng:
    def __init__(self, kb, eng, name, skip_self=False):
        self.kb = kb
        self.eng = eng
        self.name = name
        self.sem = kb.es.enter_context(kb.nc.semaphore("sem_" + name))
        self.cnt = 0
        self.seen = {}
        self.skip_self = skip_self

    def wait(self, evs):
        for sem, val in evs.items():
            if sem is self.sem and self.skip_self:
                continue
            if self.seen.get(sem, 0) < val:
                self.eng.wait_ge(sem, val)
                self.seen[sem] = val

    def op(self, fn, reads=(), writes=()):
        if self.kb.rec is not None:
            self.kb.rec.append(lambda: self._op(fn, reads, writes))
            return None
        return self._op(fn, reads, writes)

    def _op(self, fn, reads=(), writes=()):
        deps = {}
        for b in reads:
            for s, v in b.w.items():
                if deps.get(s, 0) < v:
                    deps[s] = v
        for b in writes:
            for dd in (b.w, b.r):
                for s, v in dd.items():
                    if deps.get(s, 0) < v:
                        deps[s] = v
        self.wait(deps)
        ins = fn(self.eng)
        self.cnt += 1
        ins.then_inc(self.sem, 1)
        for b in reads:
            b.r[self.sem] = self.cnt
        for b in writes:
            b.w = {self.sem: self.cnt}
            b.r = {}
        return ins


class KB:
    NDS = 40

    def __init__(self):
        self.nc = bass.Bass("TRN2", target_bir_lowering=False)
        self.es = ExitStack()
        nc = self.nc
        self.pe = Eng(self, nc.tensor, "pe", skip_self=True)
        self.act = Eng(self, nc.scalar, "act")
        self.dve = Eng(self, nc.vector, "dve")
        self.pool = Eng(self, nc.gpsimd, "pool")
        self.sp = Eng(self, nc.sync, "sp")
        self.engs = [self.pe, self.act, self.dve, self.pool, self.sp]
        self.dsems = [self.es.enter_context(nc.semaphore("dsem%d" % i)) for i in range(self.NDS)]
        self.duse = [0] * self.NDS
        self.di = 0
        self.rec = None

    def record(self):
        self.rec = []
        return self.rec

    def interleave(self, chains):
        self.rec = None
        idx = [0] * len(chains)
        live = True
        while live:
            live = False
            for i, ch in enumerate(chains):
                if idx[i] < len(ch):
                    ch[idx[i]]()
                    idx[i] += 1
                    live = True

    def dma(self, q, out, in_, reads=(), writes=()):
        if self.rec is not None:
            self.rec.append(lambda: self._dma(q, out, in_, reads, writes))
            return
        self._dma(q, out, in_, reads, writes)

    def _dma(self, q, out, in_, reads=(), writes=()):
        k = self.di % self.NDS
        self.di += 1
        sem = self.dsems[k]
        deps = {}
        if self.duse[k] > 0:
            deps[sem] = 16 * self.duse[k]
        for b in reads:
            for s, v in b.w.items():
                if deps.get(s, 0) < v:
                    deps[s] = v
        for b in writes:
            for dd in (b.w, b.r):
                for s, v in dd.items():
                    if deps.get(s, 0) < v:
                        deps[s] = v
        q.wait(deps)
        q.eng.dma_start(out=out, in_=in_).then_inc(sem, 16)
        self.duse[k] += 1
        val = 16 * self.duse[k]
        for b in reads:
            b.r[sem] = val
        for b in writes:
            b.w = {sem: val}
            b.r = {}

    def barrier(self):
        evs = {}
        for e in self.engs:
            if e.cnt > 0:
                evs[e.sem] = e.cnt
        for k in range(self.NDS):
            if self.duse[k] > 0:
                evs[self.dsems[k]] = 16 * self.duse[k]
        for e in self.engs:
            sk = e.skip_self
            e.skip_self = False
            e.wait(evs)
            e.skip_self = sk


class T:
    def __init__(self, t, name):
        self.t = t
        self.b = Buf(name)

    def __getitem__(self, idx):
        return self.t[idx]


def sbt(kb, stack, name, shape, dt):
    return T(stack.enter_context(kb.nc.sbuf_tensor(name, list(shape), dt)), name)


def pst(kb, stack, name, shape, dt):
    return T(stack.enter_context(kb.nc.psum_tensor(name, list(shape), dt)), name)


def build(cfg):
    kb = KB()
    nc = kb.nc
    pe, act, dve, pool, sp = kb.pe, kb.act, kb.dve, kb.pool, kb.sp
    S, PAST, NSB, NL, NOWN, NTOK = cfg.S, cfg.PAST, cfg.NSB, cfg.NL, cfg.NOWN, cfg.NTOK
    NS = NSB * 64
    NKBP = S // 128

    def din(name, shape, dt=F32):
        return nc.dram_tensor(name, list(shape), dt, kind="ExternalInput").ap()

    def dout(name, shape, dt=F32):
        return nc.dram_tensor(name, list(shape), dt, kind="ExternalOutput").ap()

    def dscr(name, shape, dt=BF16):
        return nc.dram_tensor(name, list(shape), dt, kind="Internal").ap()

    xf = din("xf", [S, D])
    xo = din("xo", [NOWN, D])
    xs = din("xs", [NS, D])
    csk = din("csk", [NSB, PAST, 1024])
    csv = din("csv", [NSB, PAST, 1024])
    cdk = din("cdk", [NSB, PAST, 1024])
    cdv = din("cdv", [NSB, PAST, 1024])
    w_in = din("w_in", [D, DIN])
    if "ffn" in cfg.stages:
        w_out = din("w_out", [D, D])
        w_gate = din("w_gate", [16, D, 512])
        w_up = din("w_up", [16, D, 512])
        w_down = din("w_down", [16, 512, D])
        w_r = din("w_r", [D, 20])
    NV = 20 + 4 * 128 + 2
    vecs = din("vecs", [128, NV])
    if "ffn" in cfg.stages:
        lnv = din("lnv", [128, 4 * D])
    tabs = din("tabs", [128, 128 * 4], F32)
    sbmask = din("sbmask", [128, 8, 512], BF16)
    damask = din("damask", [128, 4, 8, 512], BF16)
    sbmask_s = din("sbmask_s", [64, 512], BF16)
    damask_s = din("damask_s", [64, 512], BF16)
    atab = din("atab", [4, 72, 128], BF16)
    NKS = PAST // 128 + 1
    atab_s = din("atab_s", [4, NKS, 128], BF16)
    btab = din("btab", [4, 4, 512], BF16)
    btab_s = din("btab_s", [4, 512], BF16)
    dabias = din("dabias", [128, 4 * 72])

    y_out = dout("y_out", [NTOK, D])
    kv_p = dout("kv_p", [S, 4096])
    kv_s = dout("kv_s", [NS, 4096])

    kt_scr = dscr("kt_scr", [16, 128, S])
    v_scr = dscr("v_scr", [S, 2048])
    qt_scr = dscr("qt_scr", [16, 128, NOWN])
    kts_scr = dscr("kts_scr", [16, 128, NS])
    vs_scr = dscr("vs_scr", [NS, 2048])
    qts_scr = dscr("qts_scr", [16, 128, NS])
    if cfg.debug:
        mt_scr = dout("mt_scr", [16, 128, NTOK], BF16)
    else:
        mt_scr = dscr("mt_scr", [16, 128, NTOK])
    scrbuf = Buf("scratch")

    es = kb.es
    ident_f = sbt(kb, es, "ident_f", [128, 128], F32)
    tri_f = sbt(kb, es, "tri_f", [128, 128], F32)
    ones_f = sbt(kb, es, "ones_f", [128, 128], F32)
    ident_b = sbt(kb, es, "ident_b", [128, 128], BF16)
    ones_b = sbt(kb, es, "ones_b", [128, 128], BF16)
    vec_t = sbt(kb, es, "vec_t", [128, NV], F32)
    kb.dma(sp, ident_f[:], tabs[:, 0:128], writes=[ident_f.b])
    kb.dma(sp, tri_f[:], tabs[:, 128:256], writes=[tri_f.b])
    kb.dma(sp, ones_f[:], tabs[:, 256:384], writes=[ones_f.b])
    kb.dma(sp, vec_t[:], vecs[:, :], writes=[vec_t.b])
    dve.op(lambda e: e.tensor_copy(out=ident_b[:], in_=ident_f[:]), reads=[ident_f.b], writes=[ident_b.b])
    dve.op(lambda e: e.tensor_copy(out=ones_b[:], in_=ones_f[:]), reads=[ones_f.b], writes=[ones_b.b])
    tri_r = sbt(kb, es, "tri_r", [128, 128], F32)
    ones_r = sbt(kb, es, "ones_r", [128, 128], F32)
    dve.op(lambda e: e.tensor_copy(out=tri_r[:].bitcast(F32R), in_=tri_f[:]), reads=[tri_f.b], writes=[tri_r.b])
    dve.op(lambda e: e.tensor_copy(out=ones_r[:].bitcast(F32R), in_=ones_f[:]), reads=[ones_f.b], writes=[ones_r.b])

    def phase_proj():
        with ExitStack() as st:
            TG = 1024
            xb = [sbt(kb, st, "xb%d" % i, [128, D], BF16) for i in range(2)]
            xTs = [sbt(kb, st, "xT%d" % i, [128, 16, TG], BF16) for i in range(2)]
            wb = [sbt(kb, st, "wb%d" % i, [128, 16, 512], BF16) for i in range(2)]
            stf = [sbt(kb, st, "stf%d" % i, [128, 512], F32) for i in range(3)]
            stb = [sbt(kb, st, "stb%d" % i, [128, 512], BF16) for i in range(3)]
            tst = [sbt(kb, st, "tst%d" % i, [128, 4, TG], BF16) for i in range(2)]
            pacc = [pst(kb, st, "pacc%d" % i, [128, 512], F32) for i in range(3)]
            ptr = [pst(kb, st, "ptr%d" % i, [128, 1024], BF16) for i in range(2)]
            cnt = {"x": 0, "w": 0, "t": 0, "tr": 0, "ts": 0}

            def load_xT(src, ntok, xT):
                for tt in range(ntok // 128):
                    xbi = xb[cnt["x"] % 2]
                    cnt["x"] += 1
                    kb.dma(pool, xbi[:], src[tt * 128:(tt + 1) * 128, :], writes=[xbi.b])
                    for half in range(2):
                        p = ptr[cnt["tr"] % 2]
                        cnt["tr"] += 1
                        for j in range(8):
                            c = half * 8 + j
                            pe.op(lambda e, p=p, j=j, c=c: e.transpose(p[:, j * 128:(j + 1) * 128],
                                                                       xbi[:, c * 128:(c + 1) * 128], ident_b[:]),
                                  reads=[xbi.b, ident_b.b], writes=[p.b])
                        ev = act if (cnt["tr"] % 2) else dve
                        if ev is act:
                            ev.op(lambda e, p=p, half=half, tt=tt: e.copy(
                                out=xT[:, half * 8:(half + 1) * 8, tt * 128:(tt + 1) * 128],
                                in_=p[:].rearrange("p (c t) -> p c t", c=8)), reads=[p.b], writes=[xT.b])
                        else:
                            ev.op(lambda e, p=p, half=half, tt=tt: e.tensor_copy(
                                out=xT[:, half * 8:(half + 1) * 8, tt * 128:(tt + 1) * 128],
                                in_=p[:].rearrange("p (c t) -> p c t", c=8)), reads=[p.b], writes=[xT.b])

            def proj_cols(ntok, cts, sink_tok, sink_T, xT, hook=None):
                pend = []

                def wload(ct):
                    wbi = wb[cnt["w"] % 2]
                    cnt["w"] += 1
                    kb.dma(pool, wbi[:], w_in.rearrange("(c p) n -> p c n", p=128)[:, :, ct * 512:(ct + 1) * 512],
                           writes=[wbi.b])
                    return wbi
                wq = [wload(cts[0])]
                for ici, ct in enumerate(cts):
                    if ici == 1 and hook is not None:
                        hook()
                    wbi = wq.pop(0)
                    if ici + 1 < len(cts):
                        wq.append(wload(cts[ici + 1]))
                    kind = sink_tok(ct)
                    tsi = None
                    if kind["T"]:
                        tsi = tst[cnt["ts"] % 2]
                        cnt["ts"] += 1
                    for tt in range(ntok // 128):
                        i = cnt["t"]
                        cnt["t"] += 1
                        pa = pacc[i % 3]
                        for c in range(16):
                            pe.op(lambda e, pa=pa, c=c, tt=tt: e.matmul(pa[:], lhsT=xT[:, c, tt * 128:(tt + 1) * 128],
                                                                         rhs=wbi[:, c, :], start=(c == 0), stop=(c == 15)),
                                  reads=[xT.b, wbi.b], writes=[pa.b])
                        sf, sb_ = stf[i % 3], stb[i % 3]
                        if kind["f32"] is not None:
                            act.op(lambda e, sf=sf, pa=pa: e.copy(out=sf[:], in_=pa[:]), reads=[pa.b], writes=[sf.b])
                            kb.dma(sp, kind["f32"](tt), sf[:], reads=[sf.b], writes=[scrbuf])
                            pool.op(lambda e, sf=sf, sb_=sb_: e.tensor_copy(out=sb_[:], in_=sf[:]),
                                    reads=[sf.b], writes=[sb_.b])
                        else:
                            act.op(lambda e, sb_=sb_, pa=pa: e.activation(out=sb_[:], in_=pa[:], func=AF.Copy,
                                                                          scale=kind["scale"]),
                                   reads=[pa.b], writes=[sb_.b])
                        if kind["b16"] is not None:
                            kb.dma(sp, kind["b16"](tt), sb_[:], reads=[sb_.b], writes=[scrbuf])
                        for f in pend:
                            f()
                        pend = []
                        if kind["T"]:
                            def do_T(sb_=sb_, tsi=tsi, tt=tt):
                                p = ptr[cnt["tr"] % 2]
                                cnt["tr"] += 1
                                for j in range(4):
                                    pe.op(lambda e, j=j: e.transpose(p[:, j * 128:(j + 1) * 128],
                                                                     sb_[:, j * 128:(j + 1) * 128], ident_b[:]),
                                          reads=[sb_.b, ident_b.b], writes=[p.b])
                                dve.op(lambda e: e.tensor_copy(out=tsi[:, :, tt * 128:(tt + 1) * 128],
                                                               in_=p[:, 0:512].rearrange("p (h t) -> p h t", h=4)),
                                       reads=[p.b], writes=[tsi.b])
                            pend.append(do_T)
                    for f in pend:
                        f()
                    pend = []
                    if kind["T"]:
                        sink_T(ct, tsi)

            OUTC = {2: 0, 3: 512, 4: 1024, 5: 1536, 8: 2048, 9: 2560, 10: 3072, 11: 3584}
            KTH = {2: 0, 3: 4, 8: 8, 9: 12}
            QTH = {0: 0, 1: 4, 6: 8, 7: 12}
            VC = {4: 0, 5: 512, 10: 1024, 11: 1536}

            def mk_sinks(tok0, ntok, out_ap, kt_ap, v_ap, qt_ap, qtok0):
                def sink_tok(ct):
                    k = {"f32": None, "b16": None, "T": False, "scale": 1.0}
                    if ct in OUTC and out_ap is not None:
                        k["f32"] = lambda tt: out_ap[tok0 + tt * 128: tok0 + (tt + 1) * 128, OUTC[ct]:OUTC[ct] + 512]
                    if ct in VC and v_ap is not None:
                        k["b16"] = lambda tt: v_ap[tok0 + tt * 128: tok0 + (tt + 1) * 128, VC[ct]:VC[ct] + 512]
                    if ct in KTH and kt_ap is not None:
                        k["T"] = True
                    if ct in QTH:
                        k["T"] = True
                        k["scale"] = QSCALE
                    return k

                def sink_T(ct, tsi):
                    if ct in KTH:
                        dst, h0, t0 = kt_ap, KTH[ct], tok0
                    else:
                        dst, h0, t0 = qt_ap, QTH[ct], qtok0
                    for j in range(4):
                        kb.dma(sp, dst[h0 + j, :, t0:t0 + ntok], tsi[:, j, 0:ntok], reads=[tsi.b], writes=[scrbuf])
                return sink_tok, sink_T

            jobs = []
            for g in range(S // TG):
                jobs.append((xf[g * TG:(g + 1) * TG, :], TG, [2, 3, 4, 5, 8, 9, 10, 11],
                             mk_sinks(g * TG, TG, kv_p, kt_scr, v_scr, None, 0)))
            for g in range(NOWN // TG):
                jobs.append((xo[g * TG:(g + 1) * TG, :], TG, [0, 1, 6, 7], mk_sinks(g * TG, TG, None, None, None, qt_scr, g * TG)))
            jobs.append((xs[:, :], NS, list(range(12)), mk_sinks(0, NS, kv_s, kts_scr, vs_scr, qts_scr, 0)))
            load_xT(jobs[0][0], jobs[0][1], xTs[0])
            for k, (src, ntok, cts, (s1, s2)) in enumerate(jobs):
                hook = None
                if k + 1 < len(jobs):
                    hook = (lambda k=k: load_xT(jobs[k + 1][0], jobs[k + 1][1], xTs[(k + 1) % 2]))
                proj_cols(ntok, cts, s1, s2, xTs[k % 2], hook)
        kb.barrier()

    if "proj" in cfg.stages:
        phase_proj()

    def phase_attn():
        with ExitStack() as st:
            VOFF = 20
            lam_t = sbt(kb, st, "lam_t", [128, 8], F32)
            prod = sbt(kb, st, "lprod", [128, 128], F32)
            gsc = sbt(kb, st, "gsc", [128, 2], F32)
            for k2 in range(2):
                dve.op(lambda e, k2=k2: e.tensor_tensor(out=prod[:], in0=vec_t[:, VOFF + 256 * k2: VOFF + 256 * k2 + 128],
                                                        in1=vec_t[:, VOFF + 256 * k2 + 128: VOFF + 256 * k2 + 256],
                                                        op=ALU.mult), reads=[vec_t.b], writes=[prod.b])
                dve.op(lambda e, k2=k2: e.reduce_sum(out=lam_t[:, k2:k2 + 1], in_=prod[:], axis=AX.X),
                       reads=[prod.b], writes=[lam_t.b])
            act.op(lambda e: e.activation(out=lam_t[:, 2:4], in_=lam_t[:, 0:2], func=AF.Exp), reads=[lam_t.b], writes=[lam_t.b])
            dve.op(lambda e: e.tensor_tensor(out=lam_t[:, 4:5], in0=lam_t[:, 3:4], in1=lam_t[:, 2:3], op=ALU.subtract),
                   reads=[lam_t.b], writes=[lam_t.b])
            dve.op(lambda e: e.tensor_scalar(out=lam_t[:, 5:6], in0=lam_t[:, 4:5], scalar1=-LAM_INIT, scalar2=None,
                                             op0=ALU.add), reads=[lam_t.b], writes=[lam_t.b])
            dve.op(lambda e: e.tensor_scalar(out=gsc[:], in0=vec_t[:, VOFF + 512: VOFF + 514], scalar1=1.0 - LAM_INIT,
                                             scalar2=None, op0=ALU.mult), reads=[vec_t.b], writes=[gsc.b])
            neglam = lam_t[:, 5:6]

            sbm = sbt(kb, st, "sbm", [128, 8, 512], BF16)
            dam = sbt(kb, st, "dam", [128, 4, 8, 512], BF16)
            sbm_s = sbt(kb, st, "sbm_s", [64, 512], BF16)
            dam_s = sbt(kb, st, "dam_s", [64, 512], BF16)
            at = sbt(kb, st, "at", [4, 72, 128], BF16)
            at_s = sbt(kb, st, "at_s", [4, NKS, 128], BF16)
            bt = sbt(kb, st, "bt", [4, 4, 512], BF16)
            bt_s = sbt(kb, st, "bt_s", [4, 512], BF16)
            dab = sbt(kb, st, "dab", [128, 4 * 72], F32)
            kb.dma(sp, dab[:], dabias[:, :], writes=[dab.b])
            for dst, src in ((sbm, sbmask), (dam, damask), (sbm_s, sbmask_s), (dam_s, damask_s), (at, atab),
                             (at_s, atab_s), (bt, btab), (bt_s, btab_s)):
                kb.dma(sp, dst[:], src, writes=[dst.b])

            ez = [sbt(kb, st, "ez%d" % i, [128, 512], F32) for i in range(2)]
            spt = [sbt(kb, st, "spt%d" % i, [128, 512], F32) for i in range(3)]
            l2 = [sbt(kb, st, "l2%d" % i, [128, 512], F32) for i in range(3)]
            Sc = [sbt(kb, st, "Sc%d" % i, [128, 512], F32) for i in range(2)]
            vt = [sbt(kb, st, "vt%d" % i, [128, 512], F32) for i in range(2)]
            et = [sbt(kb, st, "et%d" % i, [128, 512], BF16) for i in range(4)]
            ot = [sbt(kb, st, "ot%d" % i, [128, 512], BF16) for i in range(2)]
            dtmp = [sbt(kb, st, "dtmp%d" % i, [128, 512], F32) for i in range(5)]
            zero_f = sbt(kb, st, "zero_f", [128, 512], F32)
            pool.op(lambda e: e.memset(zero_f[:], 0.0), writes=[zero_f.b])
            ps = [pst(kb, st, "ps%d" % i, [128, 512], F32) for i in range(8)]
            cn = {"ot": 0}

            def sb_tile(groups, blocks, W, sink):
                pz, pc, po = ps[0:2], ps[2:4], ps[4 + (cn["ot"] % 2)]
                n = len(blocks)
                for j in range(2):
                    pool.op(lambda e, j=j: e.tensor_copy(out=Sc[j][:, 0:W].bitcast(F32R), in_=zero_f[:, 0:W]),
                            reads=[zero_f.b], writes=[Sc[j].b])

                def s_qk(i):
                    kbi, nk, mk = blocks[i]
                    z = pz[i % 2]
                    ng = len(groups)
                    for gi, (KT, Vg, QT, w) in enumerate(groups):
                        pe.op(lambda e, gi=gi, KT=KT, QT=QT, w=w: e.matmul(z[0:nk, gi * w:(gi + 1) * w], lhsT=KT(kbi, nk), rhs=QT, start=(gi == 0),
                                                                         stop=(mk is None and gi == ng - 1), skip_group_check=(ng > 1)),
                              reads=[ktb, qtb], writes=[z.b])
                    if mk is not None:
                        pe.op(lambda e: e.matmul(z[0:nk, 0:W], lhsT=ident_b[0:nk, 0:nk], rhs=mk, start=False, stop=True,
                                                 skip_group_check=(ng > 1)),
                              reads=[ident_b.b, sbm.b, sbm_s.b], writes=[z.b])

                def s_ez(i):
                    kbi, nk, mk = blocks[i]
                    z, a = pz[i % 2], ez[i % 2]
                    act.op(lambda e: e.activation(out=a[0:nk, 0:W], in_=z[0:nk, 0:W], func=AF.Exp), reads=[z.b], writes=[a.b])

                def s_sp(i):
                    kbi, nk, mk = blocks[i]
                    a, b_ = ez[i % 2], spt[i % 3]
                    act.op(lambda e: e.activation(out=b_[0:nk, 0:W].bitcast(F32R), in_=a[0:nk, 0:W], func=AF.Ln, bias=1.0),
                           reads=[a.b], writes=[b_.b])

                def s_l2(i):
                    kbi, nk, mk = blocks[i]
                    z, b_, c_ = pz[i % 2], spt[i % 3], l2[i % 3]
                    dve.op(lambda e: e.scalar_tensor_tensor(out=c_[0:nk, 0:W], in0=z[0:nk, 0:W], scalar=-1.0,
                                                            in1=b_[0:nk, 0:W], op0=ALU.mult, op1=ALU.add),
                           reads=[z.b, b_.b], writes=[c_.b])

                def s_cps(i):
                    kbi, nk, mk = blocks[i]
                    c = pc[i % 2]
                    b_ = spt[i % 3]
                    s_cur, s_nxt = Sc[i % 2], Sc[(i + 1) % 2]
                    if nk == 128:
                        pe.op(lambda e: e.matmul(c[0:nk, 0:W], lhsT=tri_r[:, :].bitcast(F32R), rhs=b_[0:nk, 0:W].bitcast(F32R),
                                                 start=True, stop=False), reads=[tri_r.b, b_.b], writes=[c.b])
                        pe.op(lambda e: e.matmul(c[0:nk, 0:W], lhsT=ones_r[:, :].bitcast(F32R), rhs=s_cur[0:128, 0:W].bitcast(F32R),
                                                 start=False, stop=True), reads=[ones_r.b, s_cur.b], writes=[c.b])
                    else:
                        pe.op(lambda e: e.matmul(c[0:nk, 0:W], lhsT=tri_f[0:nk, 0:nk], rhs=b_[0:nk, 0:W], start=True, stop=False),
                              reads=[tri_f.b, b_.b], writes=[c.b])
                        pe.op(lambda e: e.matmul(c[0:nk, 0:W], lhsT=ones_f[0:128, 0:nk], rhs=s_cur[0:128, 0:W], start=False, stop=True),
                              reads=[ones_f.b, s_cur.b], writes=[c.b])
                    if i + 1 < n:
                        pool.op(lambda e: e.tensor_tensor(out=s_nxt[0:nk, 0:W].bitcast(F32R), in0=s_cur[0:nk, 0:W], in1=b_[0:nk, 0:W],
                                                          op=ALU.add), reads=[s_cur.b, b_.b], writes=[s_nxt.b])

                def s_v(i):
                    kbi, nk, mk = blocks[i]
                    c, c_, v_ = pc[i % 2], l2[i % 3], vt[i % 2]
                    dve.op(lambda e: e.tensor_tensor(out=v_[0:nk, 0:W], in0=c[0:nk, 0:W], in1=c_[0:nk, 0:W], op=ALU.add),
                           reads=[c.b, c_.b], writes=[v_.b])

                def s_e(i):
                    kbi, nk, mk = blocks[i]
                    v_, e_ = vt[i % 2], et[i % 4]
                    act.op(lambda e: e.activation(out=e_[0:nk, 0:W], in_=v_[0:nk, 0:W], func=AF.Exp, scale=-1.0),
                           reads=[v_.b], writes=[e_.b])

                def s_pv(i):
                    kbi, nk, mk = blocks[i]
                    e_ = et[i % 4]
                    ng = len(groups)
                    for gi, (KT, Vg, QT, w) in enumerate(groups):
                        pe.op(lambda e, gi=gi, Vg=Vg, w=w: e.matmul(po[0:128, gi * w:(gi + 1) * w], lhsT=Vg(kbi, nk), rhs=e_[0:nk, gi * w:(gi + 1) * w],
                                                                  start=(i == 0 and gi == 0), stop=(i == n - 1), skip_group_check=(ng > 1)),
                              reads=[vb, e_.b], writes=[po.b])

                def ok(i):
                    return 0 <= i < n

                for t in range(n + 4):
                    if ok(t):
                        s_qk(t)
                    if ok(t - 1):
                        s_sp(t - 1)
                    if ok(t - 3):
                        s_v(t - 3)
                    if ok(t):
                        s_ez(t)
                    if ok(t - 1):
                        s_l2(t - 1)
                    if ok(t - 2):
                        s_cps(t - 2)
                    if ok(t - 3):
                        s_e(t - 3)
                    if ok(t - 4):
                        s_pv(t - 4)
                o_ = ot[cn["ot"] % 2]
                cn["ot"] += 1
                act.op(lambda e: e.copy(out=o_[:, 0:W], in_=po[:, 0:W]), reads=[po.b], writes=[o_.b])
                sink(o_)

            def da_tile(groups, blocks, Bap, W, sink0, sink1, bias_of=None):
                pz = ps[0:2]
                pO = [[ps[2], ps[3]], [ps[4], ps[5]]]
                pZ = [ps[6], ps[7]]
                n = len(blocks)
                items = [(i, m) for i in range(n) for m in range(2)]

                def stA(q):
                    i, m = items[q]
                    kbi, nk, Aap, mk = blocks[i]
                    z = pz[q % 2]
                    ng = len(groups)
                    for gi, (KT0, KT1, Vg, QT0, QT1, w) in enumerate(groups):
                        pe.op(lambda e, gi=gi, KTm=(KT0, KT1)[m], QTm=(QT0, QT1)[m], w=w: e.matmul(
                            z[0:nk, gi * w:(gi + 1) * w], lhsT=KTm(kbi, nk), rhs=QTm, start=(gi == 0),
                            stop=(bias_of is not None and mk is None and gi == ng - 1), skip_group_check=(ng > 1)),
                              reads=[ktb, qtb], writes=[z.b])
                    if bias_of is None:
                        pe.op(lambda e: e.matmul(z[0:nk, 0:W], lhsT=Aap, rhs=Bap, start=False, stop=(mk is None), skip_group_check=(ng > 1)),
                              reads=[at.b, at_s.b, bt.b, bt_s.b], writes=[z.b])
                    if mk is not None:
                        pe.op(lambda e: e.matmul(z[0:nk, 0:W], lhsT=ident_b[0:nk, 0:nk], rhs=mk, start=False, stop=True,
                                                 skip_group_check=(ng > 1)),
                              reads=[ident_b.b, dam.b, dam_s.b], writes=[z.b])
                    e_ = et[q % 4]
                    if bias_of is None:
                        act.op(lambda e: e.activation(out=e_[0:nk, 0:W], in_=z[0:nk, 0:W], func=AF.Exp), reads=[z.b], writes=[e_.b])
                    else:
                        act.op(lambda e: e.activation(out=e_[0:nk, 0:W], in_=z[0:nk, 0:W], func=AF.Exp, bias=bias_of(kbi), scale=1.0),
                               reads=[z.b, dab.b], writes=[e_.b])

                def stC(q):
                    i, m = items[q]
                    kbi, nk, Aap, mk = blocks[i]
                    e_ = et[q % 4]
                    ng = len(groups)
                    for c in range(2):
                        for gi, (KT0, KT1, Vg, QT0, QT1, w) in enumerate(groups):
                            pe.op(lambda e, c=c, gi=gi, Vg=Vg, w=w: e.matmul(pO[m][c][0:128, gi * w:(gi + 1) * w], lhsT=Vg(kbi, nk, c),
                                                                           rhs=e_[0:nk, gi * w:(gi + 1) * w], start=(i == 0 and gi == 0),
                                                                           stop=(i == n - 1), skip_group_check=(ng > 1)),
                                  reads=[vb, e_.b], writes=[pO[m][c].b])
                    pe.op(lambda e: e.matmul(pZ[m][0:128, 0:W], lhsT=ones_b[0:nk, 0:128], rhs=e_[0:nk, 0:W],
                                             start=(i == 0), stop=(i == n - 1)), reads=[ones_b.b, e_.b], writes=[pZ[m].b])

                for q in range(len(items) + 1):
                    if q < len(items):
                        stA(q)
                    if q - 1 >= 0:
                        stC(q - 1)
                rz = [ez[0], ez[1]]
                for m in range(2):
                    dve.op(lambda e, m=m: e.reciprocal(out=rz[m][:, 0:W], in_=pZ[m][:, 0:W]), reads=[pZ[m].b], writes=[rz[m].b])
                oc = [dtmp[0], dtmp[1]]
                sq = [dtmp[2], dtmp[3]]
                for c in range(2):
                    t0, t1 = vt[0], vt[1]
                    dve.op(lambda e, c=c: e.tensor_tensor(out=t0[:, 0:W], in0=pO[0][c][:, 0:W], in1=rz[0][:, 0:W], op=ALU.mult),
                           reads=[pO[0][c].b, rz[0].b], writes=[t0.b])
                    dve.op(lambda e, c=c: e.tensor_tensor(out=t1[:, 0:W], in0=pO[1][c][:, 0:W], in1=rz[1][:, 0:W], op=ALU.mult),
                           reads=[pO[1][c].b, rz[1].b], writes=[t1.b])
                    dve.op(lambda e, c=c: e.scalar_tensor_tensor(out=oc[c][:, 0:W], in0=t1[:, 0:W], scalar=neglam, in1=t0[:, 0:W],
                                                                 op0=ALU.mult, op1=ALU.add),
                           reads=[t0.b, t1.b, lam_t.b], writes=[oc[c].b])
                    act.op(lambda e, c=c: e.activation(out=sq[c][:, 0:W], in_=oc[c][:, 0:W], func=AF.Square),
                           reads=[oc[c].b], writes=[sq[c].b])
                pss = ps[0]
                for c in range(2):
                    pe.op(lambda e, c=c: e.matmul(pss[0:128, 0:W], lhsT=ones_f[:, :], rhs=sq[c][:, 0:W], start=(c == 0), stop=(c == 1)),
                          reads=[ones_f.b, sq[c].b], writes=[pss.b])
                rs = dtmp[4]
                act.op(lambda e: e.activation(out=rs[:, 0:W], in_=pss[:, 0:W], func=AF.Sqrt, scale=1.0 / 256.0, bias=RMS_EPS),
                       reads=[pss.b], writes=[rs.b])
                dve.op(lambda e: e.reciprocal(out=rs[:, 0:W], in_=rs[:, 0:W]), reads=[rs.b], writes=[rs.b])
                for c in range(2):
                    o_ = ot[cn["ot"] % 2]
                    cn["ot"] += 1
                    dve.op(lambda e, c=c, o_=o_: e.scalar_tensor_tensor(out=o_[:, 0:W], in0=oc[c][:, 0:W], scalar=gsc[:, c:c + 1],
                                                                        in1=rs[:, 0:W], op0=ALU.mult, op1=ALU.mult),
                           reads=[oc[c].b, gsc.b, rs.b], writes=[o_.b])
                    (sink0, sink1)[c](o_)

            ktb, qtb, vb = Buf("kt"), Buf("qt"), Buf("v")

            def mt_sink(mc, tok0, W):
                def f(o_):
                    kb.dma(sp, mt_scr[mc, :, tok0:tok0 + W], o_[:, 0:W], reads=[o_.b], writes=[scrbuf])
                return f

            def mt_sink_multi(mcs, tok0):
                def f(o_):
                    for gi, mc in enumerate(mcs):
                        kb.dma(sp, mt_scr[mc, :, tok0:tok0 + 64], o_[:, gi * 64:(gi + 1) * 64], reads=[o_.b], writes=[scrbuf])
                return f

            if "sb" in cfg.stages or "da" in cfg.stages:
                with ExitStack() as s2:
                    kt0 = sbt(kb, s2, "kt0", [128, S], BF16)
                    kt1 = sbt(kb, s2, "kt1", [128, S], BF16)
                    qt0 = sbt(kb, s2, "qt0", [128, NOWN], BF16)
                    qt1 = sbt(kb, s2, "qt1", [128, NOWN], BF16)
                    vh = sbt(kb, s2, "vh", [128, NKBP, 256], BF16)
                    if "sb" in cfg.stages:
                        for h in range(8):
                            kb.dma(sp, kt0[:], kt_scr[h], reads=[scrbuf], writes=[ktb])
                            kb.dma(sp, qt0[:], qt_scr[h], reads=[scrbuf], writes=[qtb])
                            for k0 in range(0, NKBP, 16):
                                kb.dma(sp, vh[:, k0:k0 + 16, 0:128],
                                       v_scr[k0 * 128:(k0 + 16) * 128, h * 128:(h + 1) * 128].rearrange("(k p) d -> p k d", p=128),
                                       reads=[scrbuf], writes=[vb])
                            for l in range(NL):
                                blocks = []
                                for kbi in range(8 * l + 7, -1, -1):
                                    mk = sbm[:, kbi - 8 * l, :] if kbi >= 8 * l else None
                                    blocks.append((kbi, 128, mk))
                                sb_tile([(lambda kbi, nk: kt0[:, kbi * 128: kbi * 128 + nk],
                                          lambda kbi, nk: vh[0:nk, kbi, 0:128],
                                          qt0[:, l * 512:(l + 1) * 512], 512)], blocks, 512, mt_sink(h, l * 512, 512))
                    if "da" in cfg.stages:
                        for h in range(4):
                            kb.dma(sp, kt0[:], kt_scr[8 + 2 * h], reads=[scrbuf], writes=[ktb])
                            kb.dma(sp, kt1[:], kt_scr[9 + 2 * h], reads=[scrbuf], writes=[ktb])
                            kb.dma(sp, qt0[:], qt_scr[8 + 2 * h], reads=[scrbuf], writes=[qtb])
                            kb.dma(sp, qt1[:], qt_scr[9 + 2 * h], reads=[scrbuf], writes=[qtb])
                            for k0 in range(0, NKBP, 16):
                                kb.dma(sp, vh[:, k0:k0 + 16, :],
                                       v_scr[k0 * 128:(k0 + 16) * 128, 1024 + h * 256: 1024 + (h + 1) * 256].rearrange("(k p) d -> p k d", p=128),
                                       reads=[scrbuf], writes=[vb])
                            for l in range(NL):
                                blocks = []
                                for kbi in range(8 * l + 8):
                                    mk = dam[:, h, kbi - 8 * l, :] if kbi >= 8 * l else None
                                    blocks.append((kbi, 128, at[:, kbi - 8 * l + 64, :], mk))
                                da_tile([(lambda kbi, nk: kt0[:, kbi * 128: kbi * 128 + nk],
                                          lambda kbi, nk: kt1[:, kbi * 128: kbi * 128 + nk],
                                          lambda kbi, nk, c: vh[0:nk, kbi, c * 128:(c + 1) * 128],
                                          qt0[:, l * 512:(l + 1) * 512], qt1[:, l * 512:(l + 1) * 512], 512)], blocks, bt[:, h, :], 512,
                                        mt_sink(8 + 2 * h, l * 512, 512), mt_sink(9 + 2 * h, l * 512, 512),
                                        bias_of=(None if h == 0 else
                                                 (lambda kbi, l=l, h=h: dab[:, h * 72 + kbi - 8 * l + 64: h * 72 + kbi - 8 * l + 65])))

            kb.barrier()
            with ExitStack() as s2:
                NKC = PAST // 128
                kts = sbt(kb, s2, "kts", [128, 4, PAST + 64], BF16)
                vs = sbt(kb, s2, "vs", [128, NKC + 1, 512], BF16)
                qts = sbt(kb, s2, "qts", [128, 4, 64], BF16)
                cb = [sbt(kb, s2, "cb%d" % i, [128, 512], BF16) for i in range(2)]
                ptr = ps[7]
                ncb = {"i": 0}

                def prep(b, ck, cv, half, kt_idx0, v_col0):
                    for kbi in range(NKC):
                        c_ = cb[ncb["i"] % 2]
                        ncb["i"] += 1
                        kb.dma(pool, c_[:], ck[b, kbi * 128:(kbi + 1) * 128, half * 512:(half + 1) * 512], writes=[c_.b])
                        pt = ptr
                        for j in range(4):
                            pe.op(lambda e, j=j: e.matmul(pt[:, j * 128:(j + 1) * 128], lhsT=c_[:, j * 128:(j + 1) * 128],
                                                          rhs=ident_b[:, :], start=True, stop=True),
                                  reads=[c_.b, ident_b.b], writes=[pt.b])
                        dve.op(lambda e, kbi=kbi: e.tensor_copy(out=kts[:, :, kbi * 128:(kbi + 1) * 128],
                                                                in_=pt[:, :].rearrange("p (h t) -> p h t", h=4)),
                               reads=[pt.b], writes=[ktb])
                    for k0 in range(0, NKC, 8):
                        k1 = min(NKC, k0 + 8)
                        kb.dma(pool, vs[:, k0:k1, :],
                               cv[b, k0 * 128:k1 * 128, half * 512:(half + 1) * 512].rearrange("(k p) d -> p k d", p=128),
                               writes=[vb])
                    for j in range(4):
                        kb.dma(sp, kts[:, j, PAST:PAST + 64], kts_scr[kt_idx0 + j, :, b * 64:(b + 1) * 64], reads=[scrbuf], writes=[ktb])
                        kb.dma(sp, qts[:, j, :], qts_scr[kt_idx0 + j, :, b * 64:(b + 1) * 64], reads=[scrbuf], writes=[qtb])
                    kb.dma(sp, vs[0:64, NKC, :], vs_scr[b * 64:(b + 1) * 64, v_col0:v_col0 + 512], reads=[scrbuf], writes=[vb])

                for b in range(NSB):
                    tok0 = NOWN + b * 64
                    if "sb" in cfg.stages:
                        for half in range(2):
                            prep(b, csk, csv, half, half * 4, half * 512)
                            blocks = [(NKC, 64, sbm_s[:, 0:256])] + [(kbi, 128, None) for kbi in range(NKC - 1, -1, -1)]
                            groups = [(lambda kbi, nk, j=j: kts[:, j, kbi * 128: kbi * 128 + nk],
                                       lambda kbi, nk, j=j: vs[0:nk, kbi, j * 128:(j + 1) * 128],
                                       qts[:, j, :], 64) for j in range(4)]
                            sb_tile(groups, blocks, 256, mt_sink_multi([half * 4 + j for j in range(4)], tok0))
                    if "da" in cfg.stages:
                        for half in range(2):
                            prep(b, cdk, cdv, half, 8 + half * 4, 1024 + half * 512)
                            h0 = half * 2
                            blocks = [(kbi, 128, at_s[:, kbi, :], None) for kbi in range(NKC)]
                            blocks.append((NKC, 64, at_s[:, NKC, 0:64], dam_s[:, h0 * 64:(h0 + 2) * 64]))
                            groups = [(lambda kbi, nk, jj=jj: kts[:, 2 * jj, kbi * 128: kbi * 128 + nk],
                                       lambda kbi, nk, jj=jj: kts[:, 2 * jj + 1, kbi * 128: kbi * 128 + nk],
                                       lambda kbi, nk, c, jj=jj: vs[0:nk, kbi, jj * 256 + c * 128: jj * 256 + (c + 1) * 128],
                                       qts[:, 2 * jj, :], qts[:, 2 * jj + 1, :], 64) for jj in range(2)]
                            da_tile(groups, blocks, bt_s[:, h0 * 64:(h0 + 2) * 64], 128,
                                    mt_sink_multi([8 + 2 * (h0 + jj) for jj in range(2)], tok0),
                                    mt_sink_multi([9 + 2 * (h0 + jj) for jj in range(2)], tok0))
        kb.barrier()

    if "sb" in cfg.stages or "da" in cfg.stages:
        phase_attn()


    def phase_ffn():
        with ExitStack() as st:
            G = 512
            lnt = sbt(kb, st, "lnt", [128, 4 * D], F32)
            kb.dma(sp, lnt[:], lnv[:, :], writes=[lnt.b])
            wr_t = sbt(kb, st, "wr_t", [128, 16, 20], F32)
            kb.dma(sp, wr_t[:], w_r.rearrange("(c p) n -> p c n", p=128), writes=[wr_t.b])
            cmk = [sbt(kb, st, "cmk%d" % i, [16, G], F32) for i in range(2)]
            W = [sbt(kb, st, "W%d" % i, [128, 8192], BF16) for i in range(5)]
            mtile = sbt(kb, st, "mtile", [128, 16, G], BF16)
            u = [sbt(kb, st, "u%d" % i, [128, D], F32) for i in range(4)]
            h1Tfs = [sbt(kb, st, "h1Tf%d" % i, [128, 16, 128], F32) for i in range(2)]
            hT = [sbt(kb, st, "hT%d" % i, [128, 4, G], BF16) for i in range(2)]
            sg = [sbt(kb, st, "sg%d" % i, [128, G], F32) for i in range(2)]
            gsets = []
            for k2 in range(2):
                gsets.append(dict(
                    lg=sbt(kb, st, "lg%d" % k2, [128, 20], F32), g1=sbt(kb, st, "g1%d" % k2, [128, 16], F32),
                    oh=sbt(kb, st, "oh%d" % k2, [128, 4], F32), fs=sbt(kb, st, "fs%d" % k2, [128, 4], F32),
                    fs2=sbt(kb, st, "fs2%d" % k2, [128, 4], F32), eq1=sbt(kb, st, "eq1%d" % k2, [128, 4], F32),
                    eq2=sbt(kb, st, "eq2%d" % k2, [128, 4], F32), wi=sbt(kb, st, "wi%d" % k2, [128, 4], F32),
                    junk=sbt(kb, st, "junk%d" % k2, [128, 4], F32), comb=sbt(kb, st, "comb%d" % k2, [128, 16], F32),
                    stats=sbt(kb, st, "stats%d" % k2, [128, 4, 6], F32), mv=sbt(kb, st, "mv%d" % k2, [128, 4], F32)))
            combT = sbt(kb, st, "combT", [16, G], F32)
            ps = [pst(kb, st, "pf%d" % i, [128, 512], F32) for i in range(8)]
            pg, pu, pcb, py, ptr = ps[0:2], ps[2:4], ps[4], ps[5:7], ps[7]
            cn = {"w": 0, "acc": 0, "y": 0, "gu": 0}

            def wnext():
                w = W[cn["w"] % 5]
                cn["w"] += 1
                return w

            def layer_norm(ut, goff, gs):
                stats, mv = gs["stats"], gs["mv"]
                for k4 in range(4):
                    dve.op(lambda e, k4=k4: e.bn_stats(out=stats[:, k4, :], in_=ut[:, k4 * 512:(k4 + 1) * 512]),
                           reads=[ut.b], writes=[stats.b])
                dve.op(lambda e: e.bn_aggr(out=mv[:, 0:2], in_=stats[:].rearrange("p a b -> p (a b)")),
                       reads=[stats.b], writes=[mv.b])
                act.op(lambda e: e.activation(out=mv[:, 2:3], in_=mv[:, 1:2], func=AF.Sqrt, bias=LN_EPS, scale=1.0),
                       reads=[mv.b], writes=[mv.b])
                dve.op(lambda e: e.reciprocal(out=mv[:, 3:4], in_=mv[:, 2:3]), reads=[mv.b], writes=[mv.b])
                dve.op(lambda e: e.tensor_scalar(out=ut[:], in0=ut[:], scalar1=mv[:, 0:1], scalar2=mv[:, 3:4],
                                                 op0=ALU.subtract, op1=ALU.mult), reads=[ut.b, mv.b], writes=[ut.b])
                pool.op(lambda e: e.tensor_tensor(out=ut[:], in0=ut[:], in1=lnt[:, goff:goff + D], op=ALU.mult),
                        reads=[ut.b, lnt.b], writes=[ut.b])
                dve.op(lambda e: e.tensor_tensor(out=ut[:], in0=ut[:], in1=lnt[:, goff + D:goff + 2 * D], op=ALU.add),
                       reads=[ut.b, lnt.b], writes=[ut.b])

            def gating(tt, gs, pr, h1Tf):
                lg, g1, oh, fs, fs2, eq1, eq2, wi, junk, comb = (gs[k_] for k_ in ("lg", "g1", "oh", "fs", "fs2", "eq1", "eq2", "wi", "junk", "comb"))
                for c in range(16):
                    pe.op(lambda e, c=c: e.matmul(pr[:, 0:20], lhsT=h1Tf[:, c, :], rhs=wr_t[:, c, :], start=(c == 0), stop=(c == 15)),
                          reads=[h1Tf.b, wr_t.b], writes=[pr.b])
                R_, Wr = [lg.b, g1.b, oh.b, fs.b, fs2.b, eq1.b, eq2.b, wi.b, comb.b], None
                dve.op(lambda e: e.tensor_tensor(out=lg[:], in0=pr[:, 0:20], in1=vec_t[:, 0:20], op=ALU.add),
                       reads=[pr.b, vec_t.b], writes=[lg.b])
                dve.op(lambda e: e.reduce_max(out=g1[:, 0:1], in_=lg[:, 0:4], axis=AX.X), reads=[lg.b], writes=[g1.b])
                dve.op(lambda e: e.tensor_scalar(out=oh[:], in0=lg[:, 0:4], scalar1=g1[:, 0:1], scalar2=None, op0=ALU.is_equal),
                       reads=[lg.b, g1.b], writes=[oh.b])
                dve.op(lambda e: e.tensor_scalar(out=g1[:, 1:2], in0=g1[:, 0:1], scalar1=-1.0, scalar2=None, op0=ALU.mult),
                       reads=[g1.b], writes=[g1.b])
                act.op(lambda e: e.activation(out=junk[:], in_=lg[:, 0:4], func=AF.Exp, bias=g1[:, 1:2], scale=1.0,
                                              accum_out=g1[:, 2:3]), reads=[lg.b, g1.b], writes=[junk.b, g1.b])
                dve.op(lambda e: e.reciprocal(out=g1[:, 3:4], in_=g1[:, 2:3]), reads=[g1.b], writes=[g1.b])
                for g in range(4):
                    if g == 0:
                        dve.op(lambda e: e.tensor_scalar(out=fs[:], in0=lg[:, 4:8], scalar1=oh[:, 0:1], scalar2=None, op0=ALU.mult),
                               reads=[lg.b, oh.b], writes=[fs.b])
                    else:
                        dve.op(lambda e, g=g: e.scalar_tensor_tensor(out=fs[:], in0=lg[:, 4 + 4 * g: 8 + 4 * g], scalar=oh[:, g:g + 1],
                                                                     in1=fs[:], op0=ALU.mult, op1=ALU.add),
                               reads=[lg.b, oh.b, fs.b], writes=[fs.b])
                dve.op(lambda e: e.reduce_max(out=g1[:, 4:5], in_=fs[:], axis=AX.X), reads=[fs.b], writes=[g1.b])
                dve.op(lambda e: e.tensor_scalar(out=eq1[:], in0=fs[:], scalar1=g1[:, 4:5], scalar2=None, op0=ALU.is_equal),
                       reads=[fs.b, g1.b], writes=[eq1.b])
                dve.op(lambda e: e.scalar_tensor_tensor(out=fs2[:], in0=eq1[:], scalar=-1e30, in1=fs[:], op0=ALU.mult, op1=ALU.add),
                       reads=[eq1.b, fs.b], writes=[fs2.b])
                dve.op(lambda e: e.reduce_max(out=g1[:, 5:6], in_=fs2[:], axis=AX.X), reads=[fs2.b], writes=[g1.b])
                dve.op(lambda e: e.tensor_scalar(out=eq2[:], in0=fs2[:], scalar1=g1[:, 5:6], scalar2=None, op0=ALU.is_equal),
                       reads=[fs2.b, g1.b], writes=[eq2.b])
                dve.op(lambda e: e.tensor_tensor(out=g1[:, 6:7], in0=g1[:, 5:6], in1=g1[:, 4:5], op=ALU.subtract),
                       reads=[g1.b], writes=[g1.b])
                act.op(lambda e: e.activation(out=g1[:, 7:8], in_=g1[:, 6:7], func=AF.Exp), reads=[g1.b], writes=[g1.b])
                dve.op(lambda e: e.tensor_scalar(out=g1[:, 8:9], in0=g1[:, 7:8], scalar1=1.0, scalar2=None, op0=ALU.add),
                       reads=[g1.b], writes=[g1.b])
                dve.op(lambda e: e.reciprocal(out=g1[:, 9:10], in_=g1[:, 8:9]), reads=[g1.b], writes=[g1.b])
                dve.op(lambda e: e.tensor_tensor(out=g1[:, 10:11], in0=g1[:, 9:10], in1=g1[:, 3:4], op=ALU.mult),
                       reads=[g1.b], writes=[g1.b])
                dve.op(lambda e: e.tensor_tensor(out=g1[:, 11:12], in0=g1[:, 7:8], in1=g1[:, 10:11], op=ALU.mult),
                       reads=[g1.b], writes=[g1.b])
                dve.op(lambda e: e.tensor_scalar(out=wi[:], in0=eq1[:], scalar1=g1[:, 10:11], scalar2=None, op0=ALU.mult),
                       reads=[eq1.b, g1.b], writes=[wi.b])
                dve.op(lambda e: e.scalar_tensor_tensor(out=wi[:], in0=eq2[:], scalar=g1[:, 11:12], in1=wi[:], op0=ALU.mult, op1=ALU.add),
                       reads=[eq2.b, g1.b, wi.b], writes=[wi.b])
                for g in range(4):
                    dve.op(lambda e, g=g: e.tensor_scalar(out=comb[:, 4 * g:4 * g + 4], in0=wi[:], scalar1=oh[:, g:g + 1], scalar2=None,
                                                          op0=ALU.mult), reads=[wi.b, oh.b], writes=[comb.b])
                pe.op(lambda e: e.matmul(pr[0:16, 128:256], lhsT=comb[:, 0:16], rhs=ident_f[:, :], start=True, stop=True),
                      reads=[comb.b, ident_f.b], writes=[pr.b])
                act.op(lambda e: e.copy(out=combT[0:16, tt * 128:(tt + 1) * 128], in_=pr[0:16, 128:256]), reads=[pr.b], writes=[combT.b])

            ngroups = (NTOK + G - 1) // G
            for gi in range(ngroups):
                tok0 = gi * G
                Gc = min(G, NTOK - tok0)
                ntt = Gc // 128
                kb.dma(sp, mtile[:, :, 0:Gc], mt_scr[:, :, tok0:tok0 + Gc].rearrange("c p t -> p c t"), reads=[scrbuf], writes=[mtile.b])
                for tt in range(ntt):
                    tk = tok0 + tt * 128
                    src = xo[tk:tk + 128, :] if tk < NOWN else xs[tk - NOWN: tk - NOWN + 128, :]
                    kb.dma(sp, u[tt][:], src, writes=[u[tt].b])
                for ct in range(4):
                    w = wnext()
                    wv = w.t[:, :].rearrange("p (c n) -> p c n", c=16)
                    kb.dma(pool, wv, w_out.rearrange("(c p) n -> p c n", p=128)[:, :, ct * 512:(ct + 1) * 512], writes=[w.b])
                    for tt in range(ntt):
                        pa = py[cn["y"] % 2]
                        cn["y"] += 1
                        for c in range(16):
                            pe.op(lambda e, c=c, tt=tt, pa=pa: e.matmul(pa[:], lhsT=mtile[:, c, tt * 128:(tt + 1) * 128], rhs=wv[:, c, :],
                                                                         start=(c == 0), stop=(c == 15)),
                                  reads=[mtile.b, w.b], writes=[pa.b])
                        dve.op(lambda e, tt=tt, ct=ct, pa=pa: e.scalar_tensor_tensor(
                            out=u[tt][:, ct * 512:(ct + 1) * 512], in0=u[tt][:, ct * 512:(ct + 1) * 512], scalar=ALPHA,
                            in1=pa[:], op0=ALU.mult, op1=ALU.add), reads=[u[tt].b, pa.b], writes=[u[tt].b])
                h1Tb = mtile

                def prologue(tt, gs, ptr_, h1Tf):
                    layer_norm(u[tt], 0, gs)
                    for q4 in range(4):
                        for j in range(4):
                            c = q4 * 4 + j
                            pe.op(lambda e, c=c, j=j: e.transpose(ptr_[:, j * 128:(j + 1) * 128], u[tt][:, c * 128:(c + 1) * 128], ident_f[:]),
                                  reads=[u[tt].b, ident_f.b], writes=[ptr_.b])
                        act.op(lambda e, q4=q4: e.copy(out=h1Tf[:, q4 * 4:(q4 + 1) * 4, :], in_=ptr_[:].rearrange("p (c t) -> p c t", c=4)),
                               reads=[ptr_.b], writes=[h1Tf.b])
                    pool.op(lambda e: e.tensor_copy(out=h1Tb[:, :, tt * 128:(tt + 1) * 128], in_=h1Tf[:]),
                            reads=[h1Tf.b], writes=[h1Tb.b])
                    gating(tt, gs, ptr_, h1Tf)
                    dve.op(lambda e: e.tensor_scalar(out=u[tt][:], in0=u[tt][:], scalar1=ALPHA, scalar2=None, op0=ALU.mult),
                           reads=[u[tt].b], writes=[u[tt].b])

                for t0_ in range(0, ntt, 2):
                    chains = []
                    for k2, tt in enumerate(range(t0_, min(ntt, t0_ + 2))):
                        ch = kb.record()
                        prologue(tt, gsets[k2], (ptr, pg[0])[k2], h1Tfs[k2])
                        chains.append(ch)
                    kb.interleave(chains)
                for ex in range(16):
                    wg_, wu_, wd_ = wnext(), wnext(), wnext()
                    wgv = wg_.t[:, :].rearrange("p (c n) -> p c n", c=16)
                    wuv = wu_.t[:, :].rearrange("p (c n) -> p c n", c=16)
                    wdv = wd_.t[:, :].rearrange("p (c n) -> p c n", c=4)
                    kb.dma(pool, wgv, w_gate[ex].rearrange("(c p) n -> p c n", p=128), writes=[wg_.b])
                    kb.dma(pool, wuv, w_up[ex].rearrange("(c p) n -> p c n", p=128), writes=[wu_.b])
                    for hh in range(2):
                        kb.dma(pool, wdv[:, :, hh * 1024:(hh + 1) * 1024],
                               w_down[ex].rearrange("(c p) n -> p c n", p=128)[:, :, hh * 1024:(hh + 1) * 1024], writes=[wd_.b])
                    cm_ = cmk[ex % 2]
                    pool.op(lambda e, ex=ex, cm_=cm_: e.tensor_scalar(out=cm_[0:16, 0:Gc], in0=combT[0:16, 0:Gc], scalar1=ident_f[0:16, ex:ex + 1],
                                                                      scalar2=None, op0=ALU.mult), reads=[combT.b, ident_f.b], writes=[cm_.b])
                    pe.op(lambda e, cm_=cm_: e.matmul(pcb[:, 0:Gc], lhsT=ones_f[0:16, :], rhs=cm_[0:16, 0:Gc], start=True, stop=True),
                          reads=[ones_f.b, cm_.b], writes=[pcb.b])
                    hTe = hT[ex % 2]
                    for fc in range(4):
                        i = cn["gu"]
                        cn["gu"] += 1
                        pg_, pu_ = pg[i % 2], pu[i % 2]
                        for c in range(16):
                            pe.op(lambda e, c=c, fc=fc, pg_=pg_: e.matmul(pg_[:, 0:Gc], lhsT=wgv[:, c, fc * 128:(fc + 1) * 128], rhs=h1Tb[:, c, 0:Gc],
                                                                           start=(c == 0), stop=(c == 15)), reads=[wg_.b, h1Tb.b], writes=[pg_.b])
                        for c in range(16):
                            pe.op(lambda e, c=c, fc=fc, pu_=pu_: e.matmul(pu_[:, 0:Gc], lhsT=wuv[:, c, fc * 128:(fc + 1) * 128], rhs=h1Tb[:, c, 0:Gc],
                                                                           start=(c == 0), stop=(c == 15)), reads=[wu_.b, h1Tb.b], writes=[pu_.b])
                        sg_ = sg[i % 2]
                        hu_ = sg_
                        act.op(lambda e, sg_=sg_, pg_=pg_: e.activation(out=sg_[:, 0:Gc], in_=pg_[:, 0:Gc], func=AF.Silu),
                               reads=[pg_.b], writes=[sg_.b])
                        dve.op(lambda e, sg_=sg_, pu_=pu_, hu_=hu_: e.tensor_tensor(out=hu_[:, 0:Gc], in0=sg_[:, 0:Gc], in1=pu_[:, 0:Gc], op=ALU.mult),
                               reads=[sg_.b, pu_.b], writes=[hu_.b])
                        dve.op(lambda e, hu_=hu_, fc=fc, hTe=hTe: e.tensor_tensor(out=hTe[:, fc, 0:Gc], in0=hu_[:, 0:Gc], in1=pcb[:, 0:Gc], op=ALU.mult),
                               reads=[hu_.b, pcb.b], writes=[hTe.b])
                    for tt in range(ntt):
                        for ct in range(4):
                            pa = py[cn["y"] % 2]
                            cn["y"] += 1
                            for fc in range(4):
                                pe.op(lambda e, fc=fc, tt=tt, ct=ct, pa=pa, hTe=hTe: e.matmul(
                                    pa[:], lhsT=hTe[:, fc, tt * 128:(tt + 1) * 128], rhs=wdv[:, fc, ct * 512:(ct + 1) * 512],
                                    start=(fc == 0), stop=(fc == 3)), reads=[hTe.b, wd_.b], writes=[pa.b])
                            dve.op(lambda e, tt=tt, ct=ct, pa=pa: e.tensor_tensor(out=u[tt][:, ct * 512:(ct + 1) * 512],
                                                                                  in0=u[tt][:, ct * 512:(ct + 1) * 512], in1=pa[:], op=ALU.add),
                                   reads=[u[tt].b, pa.b], writes=[u[tt].b])
                for t0_ in range(0, ntt, 2):
                    chains = []
                    for k2, tt in enumerate(range(t0_, min(ntt, t0_ + 2))):
                        ch = kb.record()
                        layer_norm(u[tt], 2 * D, gsets[k2])
                        kb.dma(sp, y_out[tok0 + tt * 128: tok0 + (tt + 1) * 128, :], u[tt][:], reads=[u[tt].b], writes=[scrbuf])
                        chains.append(ch)
                    kb.interleave(chains)
        kb.barrier()

    if "ffn" in cfg.stages:
        phase_ffn()

    kb.barrier()
    es.close()
    return nc


def _tables(cfg, hq):
    bf = ml_dtypes.bfloat16
    p = np.arange(128)[:, None]
    f = np.arange(512)[None, :]
    tabs = np.zeros((128, 512), np.float32)
    tabs[:, 0:128] = np.eye(128, dtype=np.float32)
    jj = np.arange(128)[:, None]
    ss = np.arange(128)[None, :]
    tabs[:, 128:256] = (jj > ss).astype(np.float32)
    tabs[:, 256:384] = 1.0
    sbm = np.zeros((128, 8, 512), np.float32)
    dam = np.zeros((128, 4, 8, 512), np.float32)
    for r in range(8):
        s = 128 * r + p
        t = 512 * hq + f
        sbm[:, r, :] = np.where(s < t, 0.0, NEGM)
        vis = (s // 64) <= (t // 64)
        for h in range(4):
            corr = np.where(s > t, -2.0 * SLOPES[h] * (s - t), 0.0)
            dam[:, h, r, :] = np.where(vis, corr, NEGM)
    p64 = np.arange(64)[:, None]
    i64 = np.arange(512)[None, :] % 64
    sbm_s = np.where(p64 < i64, 0.0, NEGM).astype(np.float32)
    hcol = (np.arange(512) // 64 % 4)[None, :]
    slope_col = np.array(SLOPES, np.float32)[hcol]
    dam_s = np.where(p64 > i64, -2.0 * slope_col * (p64 - i64), 0.0).astype(np.float32)
    atab = np.zeros((4, 72, 128), np.float32)
    for j in range(72):
        rb = j - 64
        atab[0, j, :] = 1.0
        atab[1, j, :] = 1.0
        atab[2, j, :] = rb - 4 * hq
        atab[3, j, :] = np.arange(128)
    nks = cfg.PAST // 128 + 1
    atab_s = np.zeros((4, nks, 128), np.float32)
    for kbi in range(nks):
        atab_s[0, kbi, :] = 1.0
        atab_s[1, kbi, :] = 1.0
        atab_s[2, kbi, :] = kbi - (nks - 1)
        atab_s[3, kbi, :] = np.arange(128)
    btab = np.zeros((4, 4, 512), np.float32)
    tt = np.arange(512)
    for h in range(4):
        btab[0, h, :] = -SLOPES[h] * 128.0 * (tt // 128)
        btab[1, h, :] = -SLOPES[h] * (tt % 128)
        btab[2, h, :] = SLOPES[h] * 128.0
        btab[3, h, :] = SLOPES[h]
    btab_s = np.zeros((4, 512), np.float32)
    ii = np.arange(512) % 64
    sc = slope_col[0]
    btab_s[0, :] = 0.0
    btab_s[1, :] = -sc * ii
    btab_s[2, :] = sc * 128.0
    btab_s[3, :] = sc
    dabias = np.zeros((128, 4, 72), np.float32)
    for h in range(4):
        for j in range(72):
            dabias[:, h, j] = SLOPES[h] * (128.0 * (j - 64 - 4 * hq) + np.arange(128))
    return dict(dabias=dabias.reshape(128, 288), tabs=tabs, sbmask=sbm.astype(bf), damask=dam.astype(bf), sbmask_s=sbm_s.astype(bf),
                damask_s=dam_s.astype(bf), atab=atab.astype(bf), atab_s=atab_s.astype(bf),
                btab=btab.astype(bf), btab_s=btab_s.astype(bf))


def make_in_maps(cfg, inp):
    f32 = np.float32
    S, NSB = cfg.S, cfg.NSB
    x_prompt = np.asarray(inp["x_prompt"], f32)
    x_sample = np.asarray(inp["x_sample"], f32)
    w_r = np.ascontiguousarray(np.concatenate(
        [np.asarray(inp["w_coarse"], f32)[0], np.asarray(inp["w_fine"], f32)[0].reshape(D, 16)], axis=1))
    b_r = np.concatenate([np.asarray(inp["b_coarse"], f32)[0], np.asarray(inp["b_fine"], f32)[0].reshape(16)])
    sub = np.asarray(inp["subln_g"], f32)[0]
    vec_row = np.concatenate([b_r,
                              np.asarray(inp["lambda_q1"], f32)[0], np.asarray(inp["lambda_k1"], f32)[0],
                              np.asarray(inp["lambda_q2"], f32)[0], np.asarray(inp["lambda_k2"], f32)[0]])
    lnrow = np.concatenate([np.asarray(inp["ln1_g"], f32)[0], np.asarray(inp["ln1_b"], f32)[0],
                            np.asarray(inp["ln2_g"], f32)[0], np.asarray(inp["ln2_b"], f32)[0]])
    lnv = np.ascontiguousarray(np.broadcast_to(lnrow[None, :], (128, 4 * D)))
    vecs = np.zeros((128, 20 + 4 * 128 + 2), f32)
    vecs[:, :vec_row.size] = vec_row[None, :]
    vecs[:, vec_row.size] = sub[0:128]
    vecs[:, vec_row.size + 1] = sub[128:256]
    common = dict(w_in=np.ascontiguousarray(np.asarray(inp["w_in"], f32)[0]), vecs=vecs)
    if "ffn" in cfg.stages:
        common.update(
            w_out=np.ascontiguousarray(np.asarray(inp["w_out"], f32)[0]),
            w_gate=np.ascontiguousarray(np.asarray(inp["w_gate"], f32)[0]),
            w_up=np.ascontiguousarray(np.asarray(inp["w_up"], f32)[0]),
            w_down=np.ascontiguousarray(np.asarray(inp["w_down"], f32)[0]),
            w_r=w_r, lnv=lnv)
    maps = []
    for c in range(8):
        b, hq = c // 2, c % 2
        xfull = np.ascontiguousarray(x_prompt[b])
        xown = np.ascontiguousarray(xfull.reshape(S // 512, 512, D)[hq::2].reshape(S // 2, D))
        sl = slice(c * NSB, (c + 1) * NSB)
        m = dict(common)
        m.update(xf=xfull, xo=xown, xs=np.ascontiguousarray(x_sample[sl].reshape(NSB * 64, D)),
                 csk=np.ascontiguousarray(np.asarray(inp["cache_sb_k"], f32)[0, sl].reshape(NSB, cfg.PAST, 1024)),
                 csv=np.ascontiguousarray(np.asarray(inp["cache_sb_v"], f32)[0, sl].reshape(NSB, cfg.PAST, 1024)),
                 cdk=np.ascontiguousarray(np.asarray(inp["cache_da_k"], f32)[0, sl].reshape(NSB, cfg.PAST, 1024)),
                 cdv=np.ascontiguousarray(np.asarray(inp["cache_da_v"], f32)[0, sl].reshape(NSB, cfg.PAST, 1024)))
        m.update(_tables(cfg, hq))
        maps.append(m)
    return maps


def assemble(cfg, results):
    S, NSB = cfg.S, cfg.NSB
    NB = 4
    y_p = np.zeros((NB, S, D), np.float32)
    y_s = np.zeros((8 * NSB, 64, D), np.float32)
    kvp = np.zeros((NB, S, 4096), np.float32)
    kvs = np.zeros((8 * NSB, 64, 4096), np.float32)
    for c in range(8):
        b, hq = c // 2, c % 2
        r = results[c]
        yo = r["y_out"]
        y_p[b].reshape(S // 512, 512, D)[hq::2] = yo[:S // 2].reshape(S // 1024, 512, D)
        y_s[c * NSB:(c + 1) * NSB] = yo[S // 2:].reshape(NSB, 64, D)
        half = slice(hq * (S // 2), (hq + 1) * (S // 2))
        kvp[b, half] = r["kv_p"][half]
        kvs[c * NSB:(c + 1) * NSB] = r["kv_s"].reshape(NSB, 64, 4096)
    outs = (y_p, y_s,
            kvp[None, :, :, 0:1024].reshape(1, NB, S, 8, 128), kvp[None, :, :, 1024:2048].reshape(1, NB, S, 8, 128),
            kvp[None, :, :, 2048:3072].reshape(1, NB, S, 4, 2, 128), kvp[None, :, :, 3072:4096].reshape(1, NB, S, 4, 256),
            kvs[None, :, :, 0:1024].reshape(1, 8 * NSB, 64, 8, 128), kvs[None, :, :, 1024:2048].reshape(1, 8 * NSB, 64, 8, 128),
            kvs[None, :, :, 2048:3072].reshape(1, 8 * NSB, 64, 4, 2, 128), kvs[None, :, :, 3072:4096].reshape(1, 8 * NSB, 64, 4, 256))
    return tuple(np.ascontiguousarray(o) for o in outs)


def run(cfg, inp):
    nc = build(cfg)
    maps = make_in_maps(cfg, inp)
    res = run_bass_kernel_spmd(nc, maps, core_ids=list(range(8)))
    return assemble(cfg, res.results), res


def kernel(**inputs):
    cfg = Cfg()
    outs, _ = run(cfg, inputs)
    return outs
```
